# Optimizing a Trainium2 kernel written in Bass

```python
import math
import functools
import jax
import jax.numpy as jnp
from jax import lax
import numpy as np


D_MODEL = 1024
BATCH = 2
SEQ = 8192
DEPTH = 2

MLSTM_HEADS = 4
MLSTM_QK_DIM = 64
MLSTM_V_DIM = 128
MLSTM_CHUNK = 128
ATTN_HEADS = 8
ATTN_HEAD_DIM = 64
DILATED_BRANCHES = ((128, 1), (512, 4), (2048, 16))
ATTN_BLOCK = 128
REL_BUCKETS = 32
REL_MAX_DIST = 2048
CONV_CH = 512
CONV_K = 3
SGU_CH = 512
SGU_GROUPS = 4
SGU_GROUP_CH = SGU_CH // SGU_GROUPS
SGU_CHUNK = 128
D_FF = 2816
FFN_RES_W = 0.5
LN_EPS = 1e-5
DEEPNORM_ALPHA = (2 * DEPTH) ** 0.25
DEEPNORM_BETA = (8 * DEPTH) ** -0.25
N_EVEN = (DEPTH + 1) // 2
N_ODD = DEPTH // 2
MLSTM_QK_W = MLSTM_HEADS * MLSTM_QK_DIM
MLSTM_V_W = MLSTM_HEADS * MLSTM_V_DIM
ATTN_W = ATTN_HEADS * ATTN_HEAD_DIM
AB_SPLITS = (MLSTM_QK_W, MLSTM_QK_W, MLSTM_V_W, MLSTM_V_W, MLSTM_HEADS, MLSTM_HEADS, ATTN_W, ATTN_W, ATTN_W)
AB_IN_W = sum(AB_SPLITS)
AB_MIX_W = MLSTM_V_W + ATTN_W
CD_SPLITS = (CONV_CH, CONV_CH, CONV_CH, SGU_CH, SGU_CH)
CD_IN_W = sum(CD_SPLITS)
CD_MIX_W = CONV_CH + SGU_CH

kernel_name = 'hybrid_mlstm_dilated_shortconv_sgu_macaron'


def _split_cols(z, sizes):
    idx = np.cumsum(sizes)[:-1].tolist()
    return jnp.split(z, idx, axis=-1)


def layer_norm(x, g, b):
    xf = x.astype(jnp.float32)
    mu = jnp.mean(xf, axis=-1, keepdims=True)
    var = jnp.mean(jnp.square(xf - mu), axis=-1, keepdims=True)
    return ((xf - mu) * lax.rsqrt(var + LN_EPS) * g + b).astype(x.dtype)


def swiglu(h, w_gate, w_up, w_down):
    return (jax.nn.silu(h @ w_gate) * (h @ w_up)) @ w_down


def rel_bucket(dist):
    max_exact = REL_BUCKETS // 2
    d = jnp.maximum(dist, 0)
    large = max_exact + (jnp.log(jnp.maximum(d, 1).astype(jnp.float32) / max_exact)
                         / math.log(REL_MAX_DIST / max_exact) * (REL_BUCKETS - max_exact)).astype(jnp.int32)
    large = jnp.minimum(large, REL_BUCKETS - 1)
    return jnp.where(d < max_exact, d, large)


def mlstm_chunkwise(q, k, v, i_pre, f_pre):
    B, S, H, DK = q.shape
    DV = v.shape[-1]
    L = MLSTM_CHUNK
    NC = S // L
    f32 = jnp.float32
    q = q.astype(f32).reshape(B, NC, L, H, DK)
    k = (k.astype(f32) * DK ** -0.5).reshape(B, NC, L, H, DK)
    v = v.astype(f32).reshape(B, NC, L, H, DV)
    log_i = i_pre.astype(f32).reshape(B, NC, L, H).transpose(0, 1, 3, 2)
    log_f = jax.nn.log_sigmoid(f_pre.astype(f32)).reshape(B, NC, L, H).transpose(0, 1, 3, 2)
    cum_f = jnp.cumsum(log_f, axis=-1)
    causal = jnp.tril(jnp.ones((L, L), dtype=bool))
    d_log = jnp.where(causal, cum_f[..., :, None] - cum_f[..., None, :] + log_i[..., None, :], -jnp.inf)
    chunk_f = cum_f[..., -1]
    g = chunk_f[..., None] - cum_f + log_i
    g_max = jnp.max(g, axis=-1)

    def step(carry, xs):
        C, n, m = carry
        g_c, gmax_c, f_c, k_c, v_c = xs
        m_new = jnp.maximum(f_c + m, gmax_c)
        decay = jnp.exp(f_c + m - m_new)
        w = jnp.exp(g_c - m_new[..., None])
        C_new = decay[..., None, None] * C + jnp.einsum('bhl,blhk,blhv->bhkv', w, k_c, v_c)
        n_new = decay[..., None] * n + jnp.einsum('bhl,blhk->bhk', w, k_c)
        return (C_new, n_new, m_new), (C, n, m)

    init = (jnp.zeros((B, H, DK, DV), f32), jnp.zeros((B, H, DK), f32), jnp.zeros((B, H), f32))
    xs = (jnp.moveaxis(g, 1, 0), jnp.moveaxis(g_max, 1, 0), jnp.moveaxis(chunk_f, 1, 0),
          jnp.moveaxis(k, 1, 0), jnp.moveaxis(v, 1, 0))
    _, (C_prev, n_prev, m_prev) = lax.scan(step, init, xs)
    C_prev = jnp.moveaxis(C_prev, 0, 1)
    n_prev = jnp.moveaxis(n_prev, 0, 1)
    m_prev = jnp.moveaxis(m_prev, 0, 1)
    inter_log = cum_f + m_prev[..., None]
    m_t = jnp.maximum(inter_log, jnp.max(d_log, axis=-1))
    w_intra = jnp.exp(d_log - m_t[..., None]) * jnp.einsum('bcthk,bcshk->bchts', q, k)
    w_inter = jnp.exp(inter_log - m_t)
    num = (jnp.einsum('bchts,bcshv->bcthv', w_intra, v)
           + jnp.einsum('bcthk,bchkv->bcthv', q, C_prev) * w_inter.transpose(0, 1, 3, 2)[..., None])
    den = jnp.sum(w_intra, axis=-1) + jnp.einsum('bcthk,bchk->bcht', q, n_prev) * w_inter
    den = jnp.maximum(jnp.abs(den), jnp.exp(-m_t))
    h = num / den.transpose(0, 1, 3, 2)[..., None]
    return h.reshape(B, S, H, DV)


def dilated_branch(q, k, v, rel_bias, window, dilation):
    B, S, H, Dh = q.shape
    L = S // dilation
    span = window // dilation
    blk = ATTN_BLOCK
    nb = -(-L // blk)
    Lp = nb * blk

    def to_sub(t):
        t = t.reshape(B, L, dilation, H, Dh).transpose(0, 2, 1, 3, 4)
        return jnp.pad(t, ((0, 0), (0, 0), (0, Lp - L), (0, 0), (0, 0)))

    def windows(t):
        t = jnp.pad(t, ((0, 0), (0, 0), (blk, 0), (0, 0), (0, 0))).reshape(B, dilation, nb + 1, blk, H, Dh)
        return jnp.concatenate([t[:, :, :-1], t[:, :, 1:]], axis=3)

    qb = to_sub(q).reshape(B, dilation, nb, blk, H, Dh)
    kw = windows(to_sub(k))
    vw = windows(to_sub(v))
    qi = jnp.arange(blk)[:, None]
    kj = jnp.arange(2 * blk)[None, :]
    dist = qi + blk - kj
    key_idx = jnp.arange(nb)[:, None, None] * blk - blk + kj[None]
    valid = (dist >= 0) & (dist <= span) & (key_idx >= 0)
    bias = rel_bias[rel_bucket(dist * dilation)].transpose(2, 0, 1).astype(jnp.float32)
    s = jnp.einsum('brnqhd,brnkhd->brnhqk', qb, kw, preferred_element_type=jnp.float32) * (Dh ** -0.5)
    s = jnp.where(valid[None, None, :, None], s + bias, -1e30)
    m = jnp.max(s, axis=-1, keepdims=True)
    p = jnp.exp(s - m)
    l = jnp.sum(p, axis=-1, keepdims=True)
    o = jnp.einsum('brnhqk,brnkhd->brnhqd', p, vw.astype(jnp.float32)) / l
    lse = (m + jnp.log(l))[..., 0]
    o = o.transpose(0, 1, 2, 4, 3, 5).reshape(B, dilation, Lp, H, Dh)[:, :, :L]
    o = o.transpose(0, 2, 1, 3, 4).reshape(B, S, H, Dh)
    lse = lse.transpose(0, 1, 2, 4, 3).reshape(B, dilation, Lp, H)[:, :, :L]
    lse = lse.transpose(0, 2, 1, 3).reshape(B, S, H)
    return o, lse


def dilated_attention(q, k, v, rel_bias):
    outs, lses = [], []
    for window, dilation in DILATED_BRANCHES:
        o, lse = dilated_branch(q, k, v, rel_bias, window, dilation)
        outs.append(o)
        lses.append(lse)
    w = jax.nn.softmax(jnp.stack(lses, axis=0), axis=0)
    return jnp.einsum('gbsh,gbshd->bshd', w, jnp.stack(outs, axis=0))


def mixer_mlstm_dilated(h, w_in, w_out, b_igate, b_fgate, rel_bias):
    B, S, _ = h.shape
    z = h @ w_in
    mq, mk, mv, mo, mi, mf, aq, ak, av = _split_cols(z, AB_SPLITS)
    h_a = mlstm_chunkwise(mq.reshape(B, S, MLSTM_HEADS, MLSTM_QK_DIM), mk.reshape(B, S, MLSTM_HEADS, MLSTM_QK_DIM),
                          mv.reshape(B, S, MLSTM_HEADS, MLSTM_V_DIM), mi + b_igate, mf + b_fgate)
    h_a = jax.nn.sigmoid(mo.astype(jnp.float32)) * h_a.reshape(B, S, MLSTM_V_W)
    h_b = dilated_attention(aq.reshape(B, S, ATTN_HEADS, ATTN_HEAD_DIM), ak.reshape(B, S, ATTN_HEADS, ATTN_HEAD_DIM),
                            av.reshape(B, S, ATTN_HEADS, ATTN_HEAD_DIM), rel_bias).reshape(B, S, ATTN_W)
    return jnp.concatenate([h_a, h_b], axis=-1).astype(h.dtype) @ w_out


def causal_depthwise_conv(u, w, b):
    K, C = w.shape
    y = lax.conv_general_dilated(u, w[:, None, :].astype(u.dtype), window_strides=(1,), padding=[(K - 1, 0)],
                                 dimension_numbers=('NWC', 'WIO', 'NWC'), feature_group_count=C)
    return y + b


def mixer_shortconv_sgu(h, w_in, w_out, conv_w, conv_b, sgu_ln_g, sgu_ln_b, sgu_w, sgu_b):
    B, S, _ = h.shape
    z = h @ w_in
    gate_b, gate_c, xc, u, v = _split_cols(z, CD_SPLITS)
    y_c = gate_b * causal_depthwise_conv(gate_c * xc, conv_w, conv_b)
    u = jax.nn.gelu(u)
    v = jax.nn.gelu(v).reshape(B, S, SGU_GROUPS, SGU_GROUP_CH)
    v = layer_norm(v, sgu_ln_g, sgu_ln_b)
    v = v.reshape(B, S // SGU_CHUNK, SGU_CHUNK, SGU_GROUPS, SGU_GROUP_CH)
    w_s = jnp.where(jnp.tril(jnp.ones((SGU_CHUNK, SGU_CHUNK), dtype=bool)), sgu_w, 0.0)
    mixed = jnp.einsum('gts,bnsgc->bntgc', w_s.astype(v.dtype), v) + sgu_b.T[:, :, None]
    y_d = u * mixed.reshape(B, S, SGU_CH)
    return jnp.concatenate([y_c, y_d], axis=-1) @ w_out


def _ada_post_norm(x, mod, fn, ln_g, ln_b, res_w):
    shift, scale, gate = mod[:, 0, None], mod[:, 1, None], mod[:, 2, None]
    y = fn(x * (1 + scale) + shift)
    return layer_norm(DEEPNORM_ALPHA * x + res_w * (1 + gate) * y, ln_g, ln_b)


def setup_inputs(seed: int = 0) -> dict:
    key = jax.random.key(seed)
    ks = jax.random.split(key, 24)
    f32 = jnp.float32
    D = D_MODEL

    def nrm(k, shape, s):
        return jax.random.normal(k, shape, f32) * s

    return {
        'x': nrm(ks[0], (BATCH, SEQ, D), 1.0),
        'c': nrm(ks[1], (BATCH, D), 1.0),
        'rel_bias': nrm(ks[2], (REL_BUCKETS, ATTN_HEADS), 0.5),
        'ada_w': nrm(ks[3], (DEPTH, D, 9 * D), 0.2 * D ** -0.5),
        'ada_b': nrm(ks[4], (DEPTH, 9 * D), 0.02),
        'ln_g': 1.0 + nrm(ks[5], (DEPTH, 3, D), 0.02),
        'ln_b': nrm(ks[6], (DEPTH, 3, D), 0.02),
        'ffn_w_gate': nrm(ks[7], (DEPTH, 2, D, D_FF), D ** -0.5),
        'ffn_w_up': nrm(ks[8], (DEPTH, 2, D, D_FF), D ** -0.5),
        'ffn_w_down': nrm(ks[9], (DEPTH, 2, D_FF, D), DEEPNORM_BETA * D_FF ** -0.5),
        'ab_w_in': nrm(ks[10], (N_EVEN, D, AB_IN_W), D ** -0.5),
        'ab_w_out': nrm(ks[11], (N_EVEN, AB_MIX_W, D), DEEPNORM_BETA * AB_MIX_W ** -0.5),
        'ab_b_igate': nrm(ks[12], (N_EVEN, MLSTM_HEADS), 0.1),
        'ab_b_fgate': jnp.linspace(3.0, 6.0, MLSTM_HEADS, dtype=f32)[None] + nrm(ks[13], (N_EVEN, MLSTM_HEADS), 0.1),
        'cd_w_in': nrm(ks[14], (N_ODD, D, CD_IN_W), D ** -0.5),
        'cd_w_out': nrm(ks[15], (N_ODD, CD_MIX_W, D), DEEPNORM_BETA * CD_MIX_W ** -0.5),
        'cd_conv_w': nrm(ks[16], (N_ODD, CONV_K, CONV_CH), CONV_K ** -0.5),
        'cd_conv_b': nrm(ks[17], (N_ODD, CONV_CH), 0.02),
        'cd_sgu_ln_g': 1.0 + nrm(ks[18], (N_ODD, SGU_GROUPS, SGU_GROUP_CH), 0.02),
        'cd_sgu_ln_b': nrm(ks[19], (N_ODD, SGU_GROUPS, SGU_GROUP_CH), 0.02),
        'cd_sgu_w': nrm(ks[20], (N_ODD, SGU_GROUPS, SGU_CHUNK, SGU_CHUNK), SGU_CHUNK ** -0.5),
        'cd_sgu_b': 1.0 + nrm(ks[21], (N_ODD, SGU_GROUPS, SGU_CHUNK), 0.1),
    }


def reference(x, c, rel_bias, ada_w, ada_b, ln_g, ln_b, ffn_w_gate, ffn_w_up, ffn_w_down,
              ab_w_in, ab_w_out, ab_b_igate, ab_b_fgate,
              cd_w_in, cd_w_out, cd_conv_w, cd_conv_b, cd_sgu_ln_g, cd_sgu_ln_b, cd_sgu_w, cd_sgu_b):
    B = x.shape[0]
    for layer in range(DEPTH):
        mod = (jax.nn.silu(c) @ ada_w[layer] + ada_b[layer]).reshape(B, 3, 3, D_MODEL)
        ffn_pre = functools.partial(swiglu, w_gate=ffn_w_gate[layer, 0], w_up=ffn_w_up[layer, 0], w_down=ffn_w_down[layer, 0])
        ffn_post = functools.partial(swiglu, w_gate=ffn_w_gate[layer, 1], w_up=ffn_w_up[layer, 1], w_down=ffn_w_down[layer, 1])
        if layer % 2 == 0:
            e = layer // 2
            mix = functools.partial(mixer_mlstm_dilated, w_in=ab_w_in[e], w_out=ab_w_out[e],
                                    b_igate=ab_b_igate[e], b_fgate=ab_b_fgate[e], rel_bias=rel_bias)
        else:
            o = layer // 2
            mix = functools.partial(mixer_shortconv_sgu, w_in=cd_w_in[o], w_out=cd_w_out[o], conv_w=cd_conv_w[o],
                                    conv_b=cd_conv_b[o], sgu_ln_g=cd_sgu_ln_g[o], sgu_ln_b=cd_sgu_ln_b[o],
                                    sgu_w=cd_sgu_w[o], sgu_b=cd_sgu_b[o])
        x = _ada_post_norm(x, mod[:, 0], ffn_pre, ln_g[layer, 0], ln_b[layer, 0], FFN_RES_W)
        x = _ada_post_norm(x, mod[:, 1], mix, ln_g[layer, 1], ln_b[layer, 1], 1.0)
        x = _ada_post_norm(x, mod[:, 2], ffn_post, ln_g[layer, 2], ln_b[layer, 2], FFN_RES_W)
    return x
```

```python
import numpy as np
import concourse.bass as bass
import concourse.mybir as mybir
from concourse.bass_utils import run_bass_kernel_spmd

F32 = mybir.dt.float32
BF16 = mybir.dt.bfloat16
I32 = mybir.dt.int32
ALU = mybir.AluOpType
AF = mybir.ActivationFunctionType

D = 1024
DFF = 2816
NCH = DFF // 128
SEQ = 8192
NCORES = 8
TOK = 2048
NT = TOK // 128
DEPTH = 2
ALPHA = float((2 * DEPTH) ** 0.25)
LN_EPS = 1e-5
FFN_RES_W = 0.5


class Sched:
    ENGS = ("tensor", "vector", "scalar", "gpsimd", "sync")

    def __init__(self):
        self.ops = {e: [] for e in self.ENGS}
        self.cnt = {}
        self.last_w = {}
        self.readers = {}
        self.known = {e: {} for e in self.ENGS}
        self.semkeys = []

    def _sem(self, key):
        if key not in self.cnt:
            self.cnt[key] = 0
            self.semkeys.append(key)

    def _deps(self, eng, r, w):
        deps = {}
        def add(d):
            if d is None:
                return
            key, val, src = d
            if src == "tensor" and eng == "tensor":
                return
            if src == "dma":
                val = self.cnt[key]
            if deps.get(key, 0) < val:
                deps[key] = val
        for res in r:
            add(self.last_w.get(res))
        for res in w:
            add(self.last_w.get(res))
            for d in self.readers.get(res, {}).values():
                add(d)
        waits = []
        for key, val in deps.items():
            if self.known[eng].get(key, 0) >= val:
                continue
            self.known[eng][key] = val
            waits.append((key, val))
        return waits

    def _commit(self, me, r, w):
        for res in w:
            self.last_w[res] = me
            self.readers[res] = {}
        for res in r:
            self.readers.setdefault(res, {})[me[0]] = me

    @staticmethod
    def _snap(fn):
        if fn is None or fn.__closure__ is None:
            return None
        out = []
        for name, c in zip(fn.__code__.co_freevars, fn.__closure__):
            try:
                v = c.cell_contents
            except ValueError:
                continue
            if isinstance(v, (int, float, str, slice, tuple)):
                out.append((name, v))
            elif not callable(v) and not isinstance(v, (dict, list)):
                out.append((name, id(v)))
        return out

    def op(self, eng, fn, r=(), w=()):
        self.snaps = getattr(self, "snaps", {})
        self.snaps[id(fn)] = (fn, self._snap(fn))
        key = "E:" + eng
        self._sem(key)
        waits = self._deps(eng, r, w)
        self.cnt[key] += 1
        me = (key, self.cnt[key], eng)
        self._commit(me, r, w)
        self.ops[eng].append((waits, fn, key, 1))

    def dma(self, queue, fn, slot, r=(), w=()):
        self.snaps = getattr(self, "snaps", {})
        self.snaps[id(fn)] = (fn, self._snap(fn))
        key = "D:" + slot
        self._sem(key)
        waits = self._deps(queue, r, w)
        self.cnt[key] += 16
        me = (key, self.cnt[key], "dma")
        self._commit(me, r, w)
        self.ops[queue].append((waits, fn, key, 16))

    def final_waits(self, eng, keys):
        waits = [(k, self.cnt[k]) for k in keys if self.cnt.get(k, 0) > 0]
        self.ops[eng].append((waits, None, None, 0))

    def emit(self, nc):
        from contextlib import ExitStack
        for fn, snap in getattr(self, "snaps", {}).values():
            now = self._snap(fn)
            if now != snap:
                raise RuntimeError(f"late-bound closure variable in {fn.__code__.co_filename}:{fn.__code__.co_firstlineno}: {snap} -> {now}")
        with ExitStack() as es:
            sems = {}
            for k in self.semkeys:
                sems[k] = es.enter_context(nc.semaphore(k.replace(":", "_")))
            block = es.enter_context(nc.Block())

            def runner(engname):
                def body(e):
                    for waits, fn, key, inc in self.ops[engname]:
                        for wk, wv in waits:
                            e.wait_ge(sems[wk], wv)
                        if fn is not None:
                            ins = fn(e)
                            ins.then_inc(sems[key], inc)
                return body

            block.tensor(runner("tensor"))
            block.vector(runner("vector"))
            block.scalar(runner("scalar"))
            block.gpsimd(runner("gpsimd"))
            block.sync(runner("sync"))


def emit_mod(S, nc, es, cT_d, adaw_d, adab_d, ps_banks, outs, ident_deps=()):
    raise NotImplementedError


def build_ffn(res_w=FFN_RES_W, ntiles=NT):
    nc = bass.Bass("TRN2", target_bir_lowering=False)
    ntok = ntiles * 128
    x_d = nc.dram_tensor("x", [ntok, D], F32, kind="ExternalInput").ap()
    cT_d = nc.dram_tensor("cT", [128, 8], F32, kind="ExternalInput").ap()
    adaw_d = nc.dram_tensor("adaw", [6, 128, 8, 512], F32, kind="ExternalInput").ap()
    adab_d = nc.dram_tensor("adab", [128, 3 * D], F32, kind="ExternalInput").ap()
    lng_d = nc.dram_tensor("lng", [128, D], F32, kind="ExternalInput").ap()
    lnb_d = nc.dram_tensor("lnb", [128, D], F32, kind="ExternalInput").ap()
    wg_d = nc.dram_tensor("wg", [NCH, 128, 8, 128], F32, kind="ExternalInput").ap()
    wu_d = nc.dram_tensor("wu", [NCH, 128, 8, 128], F32, kind="ExternalInput").ap()
    wd_d = nc.dram_tensor("wd", [NCH, 128, D], F32, kind="ExternalInput").ap()
    ident_d = nc.dram_tensor("ident", [128, 128], F32, kind="ExternalInput").ap()
    y_d = nc.dram_tensor("y", [ntok, D], F32, kind="ExternalOutput").ap()

    from contextlib import ExitStack
    S = Sched()
    with ExitStack() as es:
        def sb(name, shape, dt):
            return es.enter_context(nc.sbuf_tensor("s_" + name, shape, dt))
        xs = sb("xs", [128, ntiles, D], F32)
        emit_ffn_body(S, nc, es, sb, xs, ntiles, res_w, cT_d, adaw_d, adab_d, lng_d, lnb_d,
                      wg_d, wu_d, wd_d, ident_d, x_d, y_d)
        S.emit(nc)
    return nc


def emit_ffn_body(S, nc, es, sb, xs, ntiles, res_w, cT_d, adaw_d, adab_d, lng_d, lnb_d,
                  wg_d, wu_d, wd_d, ident_d, x_d, y_d):
    GT = 8
    ngroups = ntiles // GT
    HCH = NCH // 2
    hT = sb("hT", [128, 8, GT * 128], BF16)
    AT = sb("AT", [128, HCH, GT * 128], BF16)
    wdS = sb("wdS", [128, HCH, D], BF16)
    NRING = 3
    wgS = [sb(f"wgS{i}", [128, 8, 128], BF16) for i in range(NRING)]
    wuS = [sb(f"wuS{i}", [128, 8, 128], BF16) for i in range(NRING)]
    sc1 = sb("sc1", [128, D], F32)
    sh = sb("sh", [128, D], F32)
    gv = sb("gv", [128, D], F32)
    lng = sb("lng", [128, D], F32)
    lnb = sb("lnb", [128, D], F32)
    adab = sb("adab", [128, 512], F32)
    adaw = [sb(f"adawS{i}", [128, 8, 512], BF16) for i in range(2)]
    cT = sb("cTs", [128, 8], F32)
    csil = sb("csil", [128, 8], F32)
    csb = sb("csb", [128, 8, 128], BF16)
    ones_bf = sb("ones_bf", [128, 128], BF16)
    identf = sb("identf", [128, 128], F32)
    ident = sb("ident", [128, 128], BF16)
    tmp = [sb(f"tmp{i}", [128, D], F32) for i in range(2)]
    hb = [sb(f"hb{i}", [128, D], BF16) for i in range(2)]
    sg = [sb(f"sg{i}", [128, 512], F32) for i in range(2)]
    st6 = sb("st6", [128, 2, 6], F32)
    mv = sb("mv", [128, 2], F32)
    rstd = sb("rstd", [128, 1], F32)
    ps = [es.enter_context(nc.psum_tensor(f"ps{i}", [128, 512], F32)) for i in range(8)]

    S.dma("sync", lambda e: e.dma_start(out=identf[:], in_=ident_d[:, :]), "c0", w=["identf"])
    S.dma("sync", lambda e: e.dma_start(out=cT[:], in_=cT_d[:, :]), "c0", w=["cT"])
    S.dma("sync", lambda e: e.dma_start(out=lng[:], in_=lng_d[:, :]), "c1", w=["lng"])
    S.dma("sync", lambda e: e.dma_start(out=lnb[:], in_=lnb_d[:, :]), "c1", w=["lnb"])
    for q in range(4):
        t0, t1 = q * ntiles // 4, (q + 1) * ntiles // 4
        S.dma("sync", lambda e, t0=t0, t1=t1: e.dma_start(
            out=xs[:, t0:t1, :], in_=x_d[t0 * 128:t1 * 128, :].rearrange("(t p) d -> p t d", p=128)),
            f"x{q}", w=[("x", t) for t in range(t0, t1)])
    S.op("vector", lambda e: e.tensor_copy(ident[:], identf[:]), r=["identf"], w=["ident"])
    S.op("vector", lambda e: e.memset(ones_bf[:], 1.0), w=["ones_bf"])
    S.op("scalar", lambda e: e.activation(csil[:], cT[:], AF.Silu), r=["cT"], w=["csil"])
    for k in range(8):
        S.op("vector", lambda e, k=k: e.tensor_scalar(csb[:, k, :], ones_bf[:], csil[:, k:k + 1], None, ALU.mult),
             r=["csil", "ones_bf"], w=[("csb", k)])
    dsts = [sh, sh, sc1, sc1, gv, gv]
    for j in range(6):
        a = adaw[j % 2]
        S.dma("gpsimd", lambda e, a=a, j=j: e.dma_start(out=a[:], in_=adaw_d[j]), f"adaw{j % 2}", w=[("adaw", j % 2)])
        S.dma("sync", lambda e, j=j: e.dma_start(out=adab[:], in_=adab_d[:, j * 512:(j + 1) * 512]), "adab", w=["adab"])
        pb = ps[j % 2]
        for k in range(8):
            S.op("tensor", lambda e, a=a, k=k, pb=pb: e.matmul(pb[:], csb[:, k, :], a[:, k, :], start=(k == 0), stop=(k == 7)),
                 r=[("csb", k), ("adaw", j % 2)], w=[("ps", j % 2)])
        dst = dsts[j][:, (j % 2) * 512:(j % 2 + 1) * 512]
        dkey = ("modt", j)
        if j < 2:
            S.op("vector", lambda e, dst=dst, pb=pb: e.tensor_tensor(dst, pb[:], adab[:], ALU.add),
                 r=[("ps", j % 2), "adab"], w=[dkey])
        elif j < 4:
            S.op("vector", lambda e, dst=dst, pb=pb: e.scalar_tensor_tensor(dst, pb[:], 1.0, adab[:], ALU.add, ALU.add),
                 r=[("ps", j % 2), "adab"], w=[dkey])
        else:
            S.op("vector", lambda e, dst=dst, pb=pb: e.scalar_tensor_tensor(dst, pb[:], 1.0, adab[:], ALU.add, ALU.add),
                 r=[("ps", j % 2), "adab"], w=[dkey])
            S.op("vector", lambda e, dst=dst: e.tensor_scalar(dst, dst, float(res_w), None, ALU.mult),
                 r=[dkey], w=[dkey])
    MODR = [("modt", j) for j in range(6)]

    chunk_order = []
    for g in range(ngroups):
        for fh in range(2):
            for fc in range(HCH):
                chunk_order.append(fh * HCH + fc)
    nload = [0]

    def load_w(i):
        if i >= len(chunk_order):
            return
        c = chunk_order[i]
        e_ = i % NRING
        S.dma("gpsimd", lambda e, c=c, e_=e_: e.dma_start(out=wgS[e_][:], in_=wg_d[c]), f"w{e_}", w=[("wg", e_)])
        S.dma("gpsimd", lambda e, c=c, e_=e_: e.dma_start(out=wuS[e_][:], in_=wu_d[c]), f"w{e_}", w=[("wu", e_)])

    for i in range(NRING):
        load_w(i)

    mvg = sb("mvg", [128, GT, 2], F32)
    st6g = sb("st6g", [128, GT, 2, 6], F32)
    rstdg = sb("rstdg", [128, GT], F32)

    def emit_step1(g):
        for tt in range(GT):
            t = g * GT + tt
            tm, hbt = tmp[tt % 2], hb[tt % 2]
            pT = ps[2 + (tt % 2)]
            pTb = pT[:].bitcast(BF16)
            S.op("vector", lambda e, tm=tm, t=t: e.tensor_tensor(tm[:], xs[:, t, :], sc1[:], ALU.mult),
                 r=[("x", t)] + MODR, w=[("tmp", tt % 2)])
            S.op("gpsimd", lambda e, tm=tm, hbt=hbt: e.tensor_tensor(hbt[:], tm[:], sh[:], ALU.add),
                 r=[("tmp", tt % 2)] + MODR, w=[("hb", tt % 2)])
            for k in range(8):
                S.op("tensor", lambda e, k=k, hbt=hbt, pTb=pTb: e.transpose(pTb[:, k * 128:(k + 1) * 128], hbt[:, k * 128:(k + 1) * 128], ident[:]),
                     r=[("hb", tt % 2), "ident"], w=[("ps", 2 + (tt % 2))])
            S.op("scalar", lambda e, tt=tt, pTb=pTb: e.activation(
                hT[:, :, tt * 128:(tt + 1) * 128], pTb.rearrange("p (k t) -> p k t", k=8), AF.Copy),
                r=[("ps", 2 + (tt % 2))], w=[("hT", tt)])

    def emit_ln_tail(g):
        MVG = [("mvg", tt) for tt in range(GT)]
        S.op("vector", lambda e: e.tensor_scalar(rstdg[:], mvg[:, :, 1], LN_EPS, None, ALU.add), r=MVG, w=["rstdg"])
        S.op("scalar", lambda e: e.activation(rstdg[:], rstdg[:], AF.Sqrt), r=["rstdg"], w=["rstdg"])
        S.op("vector", lambda e: e.reciprocal(rstdg[:], rstdg[:]), r=["rstdg"], w=["rstdg"])
        for tt in range(GT):
            t = g * GT + tt
            xap = xs[:, t, :]
            S.op("vector", lambda e, xap=xap, tt=tt: e.tensor_scalar(xap, xap, mvg[:, tt, 0:1], rstdg[:, tt:tt + 1], ALU.subtract, ALU.mult),
                 r=[("mvg", tt), "rstdg", ("x", t)], w=[("x", t)])
            S.op("gpsimd", lambda e, xap=xap: e.tensor_tensor(xap, xap, lng[:], ALU.mult), r=[("x", t), "lng"], w=[("x", t)])
            S.op("gpsimd", lambda e, xap=xap: e.tensor_tensor(xap, xap, lnb[:], ALU.add), r=[("x", t), "lnb"], w=[("x", t)])
            S.dma("sync", lambda e, t=t: e.dma_start(out=y_d[t * 128:(t + 1) * 128, :], in_=xs[:, t, :]),
                  f"y{t % 4}", r=[("x", t)])

    ci = 0
    HTR = [("hT", tt) for tt in range(GT)]
    emit_step1(0)
    for g in range(ngroups):
        for fh in range(2):
            for fc in range(HCH):
                c = fh * HCH + fc
                S.dma("gpsimd", lambda e, c=c, fc=fc: e.dma_start(out=wdS[:, fc, :], in_=wd_d[c]), "wd", w=["wd"])
            for fc in range(HCH):
                e_ = ci % NRING
                for hf in range(2):
                    slot = (fc * 2 + hf) % 2
                    pG, pU = ps[4 + slot * 2], ps[5 + slot * 2]
                    for k in range(8):
                        S.op("tensor", lambda e, k=k, e_=e_, hf=hf, pG=pG: e.matmul(
                            pG[:], wgS[e_][:, k, :], hT[:, k, hf * 512:(hf + 1) * 512], start=(k == 0), stop=(k == 7)),
                            r=[("wg", e_)] + HTR[hf * 4:(hf + 1) * 4], w=[("ps", 4 + slot * 2)])
                    for k in range(8):
                        S.op("tensor", lambda e, k=k, e_=e_, hf=hf, pU=pU: e.matmul(
                            pU[:], wuS[e_][:, k, :], hT[:, k, hf * 512:(hf + 1) * 512], start=(k == 0), stop=(k == 7)),
                            r=[("wu", e_)] + HTR[hf * 4:(hf + 1) * 4], w=[("ps", 5 + slot * 2)])
                    sgt = sg[slot]
                    S.op("scalar", lambda e, sgt=sgt, pG=pG: e.activation(sgt[:], pG[:], AF.Silu),
                         r=[("ps", 4 + slot * 2)], w=[("sg", slot)])
                    S.op("vector", lambda e, sgt=sgt, pU=pU, fc=fc, hf=hf: e.tensor_tensor(
                        AT[:, fc, hf * 512:(hf + 1) * 512], sgt[:], pU[:], ALU.mult),
                        r=[("sg", slot), ("ps", 5 + slot * 2)], w=[("AT", fc, hf)])
                load_w(ci + NRING)
                ci += 1
            for tt in range(GT):
                t = g * GT + tt
                hf = tt // 4
                pa = (tt % 2) * 2
                for dh in range(2):
                    pY = ps[pa + dh]
                    for fc in range(HCH):
                        S.op("tensor", lambda e, fc=fc, tt=tt, dh=dh, pY=pY: e.matmul(
                            pY[:], AT[:, fc, tt * 128:(tt + 1) * 128], wdS[:, fc, dh * 512:(dh + 1) * 512],
                            start=(fc == 0), stop=(fc == HCH - 1)),
                            r=[("AT", fc, hf), "wd"], w=[("ps", pa + dh)])
                tm = tmp[tt % 2]
                for dh in range(2):
                    S.op("vector", lambda e, tm=tm, dh=dh, pa=pa: e.tensor_tensor(
                        tm[:, dh * 512:(dh + 1) * 512], ps[pa + dh][:], gv[:, dh * 512:(dh + 1) * 512], ALU.mult),
                        r=[("ps", pa + dh)] + MODR, w=[("tmp", tt % 2, dh)])
                TM = [("tmp", tt % 2, 0), ("tmp", tt % 2, 1), ("tmp", tt % 2)]
                if fh == 0:
                    S.op("vector", lambda e, tm=tm, t=t: e.scalar_tensor_tensor(
                        xs[:, t, :], xs[:, t, :], ALPHA, tm[:], ALU.mult, ALU.add),
                        r=TM + [("x", t)], w=[("x", t), ("tmp", tt % 2)])
                else:
                    S.op("vector", lambda e, tm=tm, t=t: e.tensor_tensor(xs[:, t, :], xs[:, t, :], tm[:], ALU.add),
                         r=TM + [("x", t)], w=[("x", t), ("tmp", tt % 2)])
                    for hcol in range(2):
                        S.op("vector", lambda e, hcol=hcol, t=t, tt=tt: e.bn_stats(st6g[:, tt, hcol, :], xs[:, t, hcol * 512:(hcol + 1) * 512]),
                             r=[("x", t)], w=[("st6g", tt, hcol)])
                    S.op("vector", lambda e, tt=tt: e.bn_aggr(mvg[:, tt, :], st6g[:, tt, :, :]),
                         r=[("st6g", tt, 0), ("st6g", tt, 1)], w=[("mvg", tt)])
        if g + 1 < ngroups:
            emit_step1(g + 1)
        emit_ln_tail(g)
    S.final_waits("sync", [f"D:y{i}" for i in range(4)])


def emit_ln(S, xap, xkey, st6, mv, rstd, lng, lnb, eng="vector"):
    for hcol in range(2):
        S.op("vector", lambda e, hcol=hcol: e.bn_stats(st6[:, hcol, :], xap[:, hcol * 512:(hcol + 1) * 512]),
             r=[xkey], w=[("st6", hcol)])
    S.op("vector", lambda e: e.bn_aggr(mv[:], st6[:]), r=[("st6", 0), ("st6", 1)], w=["mv"])
    S.op("vector", lambda e: e.tensor_scalar(rstd[:], mv[:, 1:2], LN_EPS, None, ALU.add), r=["mv"], w=["rstd"])
    S.op("scalar", lambda e: e.activation(rstd[:], rstd[:], AF.Sqrt), r=["rstd"], w=["rstd"])
    S.op("vector", lambda e: e.reciprocal(rstd[:], rstd[:]), r=["rstd"], w=["rstd"])
    S.op("vector", lambda e: e.tensor_scalar(xap, xap, mv[:, 0:1], rstd[:, 0:1], ALU.subtract, ALU.mult),
         r=["mv", "rstd", xkey], w=[xkey])
    S.op("vector", lambda e: e.tensor_tensor(xap, xap, lng[:], ALU.mult), r=[xkey, "lng"], w=[xkey])
    S.op("vector", lambda e: e.tensor_tensor(xap, xap, lnb[:], ALU.add), r=[xkey, "lnb"], w=[xkey])


def lay_w_in(w, mc=128):
    F = w.shape[1]
    return np.ascontiguousarray(w.reshape(8, 128, F // mc, mc).transpose(2, 1, 0, 3))


def lay_adaw(w):
    return np.ascontiguousarray(w.reshape(8, 128, 6, 512).transpose(2, 1, 0, 3))


def ffn_inputs(x_sh, c_row, ada_w_l, ada_b_l, s, lng, lnb, wg, wu, wd):
    sl = slice(3 * s * D, 3 * (s + 1) * D)
    return {
        "x": np.ascontiguousarray(x_sh),
        "cT": np.ascontiguousarray(c_row.reshape(8, 128).T),
        "adaw": lay_adaw(ada_w_l[:, sl]),
        "adab": np.ascontiguousarray(np.broadcast_to(ada_b_l[sl][None, :], (128, 3 * D))),
        "lng": np.ascontiguousarray(np.broadcast_to(lng[None, :], (128, D))),
        "lnb": np.ascontiguousarray(np.broadcast_to(lnb[None, :], (128, D))),
        "wg": lay_w_in(wg), "wu": lay_w_in(wu),
        "wd": np.ascontiguousarray(wd.reshape(NCH, 128, D)),
        "ident": np.eye(128, dtype=np.float32),
    }


def emit_consts(S, sb, ident_d):
    identf = sb("identf", [128, 128], F32)
    ident = sb("ident", [128, 128], BF16)
    ones_bf = sb("ones_bf", [128, 128], BF16)
    S.dma("sync", lambda e: e.dma_start(out=identf[:], in_=ident_d[:, :]), "identf", w=["identf"])
    S.op("vector", lambda e: e.tensor_copy(ident[:], identf[:]), r=["identf"], w=["ident"])
    S.op("vector", lambda e: e.memset(ones_bf[:], 1.0), w=["ones_bf"])
    return ident, ones_bf


def emit_mod3(S, sb, ps, ones_bf, cT_d, adaw_d, adab_d, res_w, stage=None):
    sc1 = sb("sc1", [128, D], F32)
    sh = sb("sh", [128, D], F32)
    gv = sb("gv", [128, D], F32)
    adab = sb("adabS", [128, 512], F32)
    if stage is None:
        adaw_t = [sb(f"adawS{i}", [128, 8, 512], BF16) for i in range(2)]
        adaw = [t[:] for t in adaw_t]
        akeys = [("adaw", 0), ("adaw", 1)]
    else:
        adaw = [st[0] for st in stage]
        akeys = [st[1] for st in stage]
    cT = sb("cTs", [128, 8], F32)
    csil = sb("csil", [128, 8], F32)
    csb = sb("csb", [128, 8, 128], BF16)
    S.dma("sync", lambda e: e.dma_start(out=cT[:], in_=cT_d[:, :]), "cT", w=["cT"])
    S.op("scalar", lambda e: e.activation(csil[:], cT[:], AF.Silu), r=["cT"], w=["csil"])
    for k in range(8):
        S.op("vector", lambda e, k=k: e.tensor_scalar(csb[:, k, :], ones_bf[:], csil[:, k:k + 1], None, ALU.mult),
             r=["csil", "ones_bf"], w=[("csb", k)])
    dsts = [sh, sh, sc1, sc1, gv, gv]
    for j in range(6):
        a = adaw[j % 2]
        S.dma("gpsimd", lambda e, a=a, j=j: e.dma_start(out=a, in_=adaw_d[j]), f"adaw{j % 2}", w=[akeys[j % 2]])
        S.dma("sync", lambda e, j=j: e.dma_start(out=adab[:], in_=adab_d[:, j * 512:(j + 1) * 512]), "adab", w=["adab"])
        pb = ps[j % 2]
        for k in range(8):
            S.op("tensor", lambda e, a=a, k=k, pb=pb: e.matmul(pb[:], csb[:, k, :], a[:, k, :], start=(k == 0), stop=(k == 7)),
                 r=[("csb", k), akeys[j % 2]], w=[("ps", j % 2)])
        dst = dsts[j][:, (j % 2) * 512:(j % 2 + 1) * 512]
        dkey = ("modt", j)
        if j < 2:
            S.op("vector", lambda e, dst=dst, pb=pb: e.tensor_tensor(dst, pb[:], adab[:], ALU.add),
                 r=[("ps", j % 2), "adab"], w=[dkey])
        else:
            S.op("vector", lambda e, dst=dst, pb=pb: e.scalar_tensor_tensor(dst, pb[:], 1.0, adab[:], ALU.add, ALU.add),
                 r=[("ps", j % 2), "adab"], w=[dkey])
            if j >= 4 and res_w != 1.0:
                S.op("vector", lambda e, dst=dst: e.tensor_scalar(dst, dst, float(res_w), None, ALU.mult),
                     r=[dkey], w=[dkey])
    return sh, sc1, gv, [("modt", j) for j in range(6)]


def emit_h_transpose(S, xap, xkey, tmp, hb, slot, sc1, sh, MODR, ident, pT, pskey, dst_fn, dkey):
    tm, hbt = tmp[slot], hb[slot]
    pTb = pT[:].bitcast(BF16)
    S.op("vector", lambda e: e.tensor_tensor(tm[:], xap, sc1[:], ALU.mult), r=[xkey] + MODR, w=[("tmp", slot)])
    S.op("gpsimd", lambda e: e.tensor_tensor(hbt[:], tm[:], sh[:], ALU.add), r=[("tmp", slot)] + MODR, w=[("hb", slot)])
    for k in range(8):
        S.op("tensor", lambda e, k=k: e.transpose(pTb[:, k * 128:(k + 1) * 128], hbt[:, k * 128:(k + 1) * 128], ident[:]),
             r=[("hb", slot), "ident"], w=[pskey])
    S.op("scalar", lambda e: e.activation(dst_fn(), pTb.rearrange("p (k t) -> p k t", k=8), AF.Copy),
         r=[pskey], w=[dkey])


def emit_gelu(S, z, zkeys, out, outkeys, s1, s1key, n):
    S.op("scalar", lambda e: e.activation(s1[:, :n], z, AF.Square), r=zkeys, w=[s1key])
    S.op("vector", lambda e: e.tensor_scalar(s1[:, :n], s1[:, :n], 0.044715, 1.0, ALU.mult, ALU.add), r=[s1key], w=[s1key])
    S.op("vector", lambda e: e.tensor_tensor(s1[:, :n], s1[:, :n], z, ALU.mult), r=[s1key] + zkeys, w=[s1key])
    S.op("scalar", lambda e: e.activation(s1[:, :n], s1[:, :n], AF.Sigmoid, scale=1.5957691216057308), r=[s1key], w=[s1key])
    S.op("vector", lambda e: e.tensor_tensor(out, s1[:, :n], z, ALU.mult), r=[s1key] + zkeys, w=outkeys)


def emit_out_proj_ln(S, sb, ps, lhs_fn, lhs_keys_fn, woS, wokey, gv, MODR, lng, lnb, x_d, y_d, ntiles, tmp, xin, st6, mv, rstd):
    for t in range(ntiles):
        xt = xin[t % 2]
        S.dma("sync", lambda e, t=t, xt=xt: e.dma_start(out=xt[:], in_=x_d[t * 128:(t + 1) * 128, :]), f"xin{t % 2}", w=[("xin", t % 2)])
        pa = (t % 2) * 2
        for dh in range(2):
            for k in range(8):
                S.op("tensor", lambda e, t=t, dh=dh, k=k, pa=pa: e.matmul(
                    ps[pa + dh][:], lhs_fn(k, t), woS[:, k, dh * 512:(dh + 1) * 512], start=(k == 0), stop=(k == 7)),
                    r=lhs_keys_fn(k, t) + [wokey], w=[("ps", pa + dh)])
        tm = tmp[t % 2]
        for dh in range(2):
            S.op("vector", lambda e, tm=tm, dh=dh, pa=pa: e.tensor_tensor(
                tm[:, dh * 512:(dh + 1) * 512], ps[pa + dh][:], gv[:, dh * 512:(dh + 1) * 512], ALU.mult),
                r=[("ps", pa + dh)] + MODR, w=[("tmp", t % 2)])
        S.op("vector", lambda e, tm=tm, xt=xt: e.scalar_tensor_tensor(xt[:], xt[:], ALPHA, tm[:], ALU.mult, ALU.add),
             r=[("tmp", t % 2), ("xin", t % 2)], w=[("xin", t % 2)])
        emit_ln(S, xt[:], ("xin", t % 2), st6, mv, rstd, lng, lnb)
        S.dma("sync", lambda e, t=t, xt=xt: e.dma_start(out=y_d[t * 128:(t + 1) * 128, :], in_=xt[:]), f"yo{t % 2}", r=[("xin", t % 2)])
    S.final_waits("sync", ["D:yo0", "D:yo1"])


def build_cd():
    nc = bass.Bass("TRN2", target_bir_lowering=False)
    NTH = NT + 1
    x_d = nc.dram_tensor("x", [TOK, D], F32, kind="ExternalInput").ap()
    xh_d = nc.dram_tensor("xh", [128, D], F32, kind="ExternalInput").ap()
    flag_d = nc.dram_tensor("flag", [128, 1], F32, kind="ExternalInput").ap()
    cT_d = nc.dram_tensor("cT", [128, 8], F32, kind="ExternalInput").ap()
    adaw_d = nc.dram_tensor("adaw", [6, 128, 8, 512], F32, kind="ExternalInput").ap()
    adab_d = nc.dram_tensor("adab", [128, 3 * D], F32, kind="ExternalInput").ap()
    lng_d = nc.dram_tensor("lng", [128, D], F32, kind="ExternalInput").ap()
    lnb_d = nc.dram_tensor("lnb", [128, D], F32, kind="ExternalInput").ap()
    win_d = nc.dram_tensor("win", [16, 128, 8, 128], F32, kind="ExternalInput").ap()
    wv_d = nc.dram_tensor("wv", [128, 8, 512], F32, kind="ExternalInput").ap()
    wo_d = nc.dram_tensor("wo", [128, 8, D], F32, kind="ExternalInput").ap()
    cw_d = nc.dram_tensor("cw", [128, 4, 4], F32, kind="ExternalInput").ap()
    sg_d = nc.dram_tensor("sgg", [128, 512], F32, kind="ExternalInput").ap()
    sbb_d = nc.dram_tensor("sgb", [128, 512], F32, kind="ExternalInput").ap()
    wm_d = nc.dram_tensor("wm", [128, 4, 128], F32, kind="ExternalInput").ap()
    msk_d = nc.dram_tensor("msk", [128, 128], F32, kind="ExternalInput").ap()
    sgub_d = nc.dram_tensor("sgub", [128, 4, 512], F32, kind="ExternalInput").ap()
    ident_d = nc.dram_tensor("ident", [128, 128], F32, kind="ExternalInput").ap()
    y_d = nc.dram_tensor("y", [TOK, D], F32, kind="ExternalOutput").ap()

    from contextlib import ExitStack
    S = Sched()
    with ExitStack() as es:
        def sb(name, shape, dt):
            return es.enter_context(nc.sbuf_tensor("s_" + name, shape, dt))
        ps = [es.enter_context(nc.psum_tensor(f"ps{i}", [128, 512], F32)) for i in range(8)]
        ident, ones_bf = emit_consts(S, sb, ident_d)
        acc = sb("acc", [128, TOK], F32)
        gu = sb("gu", [128, TOK], F32)
        stage = [(acc[:].bitcast(BF16).rearrange("p (k c) -> p k c", k=8), "acc"),
                 (gu[:].bitcast(BF16).rearrange("p (k c) -> p k c", k=8), ("gu", 0))]
        sh, sc1, gv, MODR = emit_mod3(S, sb, ps, ones_bf, cT_d, adaw_d, adab_d, 1.0, stage=stage)
        lng = sb("lngS", [128, D], F32)
        lnb = sb("lnbS", [128, D], F32)
        S.dma("sync", lambda e: e.dma_start(out=lng[:], in_=lng_d[:, :]), "lng", w=["lng"])
        S.dma("sync", lambda e: e.dma_start(out=lnb[:], in_=lnb_d[:, :]), "lnb", w=["lnb"])
        hT = sb("hT", [128, 8, NTH * 128], BF16)
        ycT = sb("ycT", [128, 4, TOK], BF16)
        ydT = sb("ydT", [128, 4, TOK], BF16)
        vn = sb("vn", [128, NT, 512], BF16)
        woS = sb("woS", [128, 8, D], BF16)
        wvS = sb("wvS", [128, 8, 512], BF16)
        NR = 3
        wr = [sb(f"wr{i}", [128, 8, 128], BF16) for i in range(NR)]
        pT_ = sb("pT", [128, 128 + TOK], F32)
        s1 = [sb(f"s1_{i}", [128, 512], F32) for i in range(2)]
        gcs = [sb(f"gcs{i}", [128, 128], F32) for i in range(1)]
        vg = [sb(f"vg{i}", [128, 512], F32) for i in range(2)]
        tmp = [sb(f"tmp{i}", [128, D], F32) for i in range(2)]
        hb = [sb(f"hb{i}", [128, D], BF16) for i in range(2)]
        xin = [sb(f"xin{i}", [128, D], F32) for i in range(2)]
        cw = sb("cwS", [128, 4, 4], F32)
        flag = sb("flagS", [128, 1], F32)
        sgg = sb("sggS", [128, 512], F32)
        sgb = sb("sgbS", [128, 512], F32)
        wmf = sb("wmf", [128, 4, 128], F32)
        mskS = sb("mskS", [128, 128], F32)
        wmT = sb("wmT", [128, 4, 128], BF16)
        sgub = sb("sgubS", [128, 4, 512], F32)
        st6 = sb("st6", [128, 4, 6], F32)
        mv = sb("mv", [128, 4, 2], F32)
        rstd = sb("rstd", [128, 4], F32)

        for (t_, d_, nm) in [(cw, cw_d, "cw"), (flag, flag_d, "flag"), (sgg, sg_d, "sgg"), (sgb, sbb_d, "sgb"),
                             (wmf, wm_d, "wmf"), (mskS, msk_d, "msk"), (sgub, sgub_d, "sgub")]:
            S.dma("sync", lambda e, t_=t_, d_=d_: e.dma_start(out=t_[:], in_=d_), nm, w=[nm])
        S.dma("gpsimd", lambda e: e.dma_start(out=wvS[:], in_=wv_d), "wv", w=["wv"])
        S.dma("gpsimd", lambda e: e.dma_start(out=woS[:], in_=wo_d), "wo", w=["wo"])
        for g in range(4):
            S.op("vector", lambda e, g=g: e.tensor_tensor(wmT[:, g, :], wmf[:, g, :], mskS[:], ALU.mult),
                 r=["wmf", "msk"], w=[("wmT", g)])

        for i in range(NTH):
            xt = xin[i % 2]
            src = xh_d if i == 0 else x_d[(i - 1) * 128:i * 128, :]
            S.dma("sync", lambda e, xt=xt, src=src: e.dma_start(out=xt[:], in_=src), f"xin{i % 2}", w=[("xin", i % 2)])
            emit_h_transpose(S, xt[:], ("xin", i % 2), tmp, hb, i % 2, sc1, sh, MODR, ident, ps[2 + i % 2], ("ps", 2 + i % 2),
                             (lambda i=i: hT[:, :, i * 128:(i + 1) * 128]), ("hT", i))
        HT_ALL = [("hT", i) for i in range(NTH)]

        def piece_keys(pc):
            return [("hT", 1 + pc * 4 + a) for a in range(4)]

        for t in range(NT):
            pv = ps[4 + t % 2]
            for k in range(8):
                S.op("tensor", lambda e, t=t, k=k, pv=pv: e.matmul(pv[:], hT[:, k, (t + 1) * 128:(t + 2) * 128], wvS[:, k, :],
                                                              start=(k == 0), stop=(k == 7)),
                     r=[("hT", t + 1), "wv"], w=[("ps", 4 + t % 2)])
            vgt = vg[t % 2]
            emit_gelu(S, pv[:], [("ps", 4 + t % 2)], vgt[:], [("vg", t % 2)], s1[t % 2], ("s1", t % 2), 512)
            for g in range(4):
                S.op("vector", lambda e, g=g, vgt=vgt: e.bn_stats(st6[:, g, :], vgt[:, g * 128:(g + 1) * 128]),
                     r=[("vg", t % 2)], w=[("st6", g)])
                S.op("vector", lambda e, g=g: e.bn_aggr(mv[:, g, :], st6[:, g, :]), r=[("st6", g)], w=[("mv", g)])
            MV = [("mv", g) for g in range(4)]
            S.op("vector", lambda e: e.tensor_scalar(rstd[:], mv[:, :, 1], LN_EPS, None, ALU.add), r=MV, w=["rstd"])
            S.op("scalar", lambda e: e.activation(rstd[:], rstd[:], AF.Sqrt), r=["rstd"], w=["rstd"])
            S.op("vector", lambda e: e.reciprocal(rstd[:], rstd[:]), r=["rstd"], w=["rstd"])
            for g in range(4):
                S.op("vector", lambda e, g=g, vgt=vgt: e.tensor_scalar(vgt[:, g * 128:(g + 1) * 128], vgt[:, g * 128:(g + 1) * 128],
                                                                    mv[:, g, 0:1], rstd[:, g:g + 1], ALU.subtract, ALU.mult),
                     r=[("vg", t % 2), "rstd"] + MV, w=[("vg", t % 2)])
            S.op("vector", lambda e, vgt=vgt: e.tensor_tensor(vgt[:], vgt[:], sgg[:], ALU.mult), r=[("vg", t % 2), "sgg"], w=[("vg", t % 2)])
            S.op("vector", lambda e, vgt=vgt, t=t: e.tensor_tensor(vn[:, t, :], vgt[:], sgb[:], ALU.add),
                 r=[("vg", t % 2), "sgb"], w=[("vn", t)])

        order = []
        for q in range(4):
            order += [4 + q, 8 + q, q]
        for g in range(4):
            order.append(12 + g)
        state = {"i": 0}

        def load_next():
            i = state["i"]
            if i >= len(order):
                return
            c = order[i]
            S.dma("gpsimd", lambda e, c=c, i=i: e.dma_start(out=wr[i % NR][:], in_=win_d[c]), f"wr{i % NR}", w=[("wr", i % NR)])
            state["i"] += 1

        for _ in range(NR):
            load_next()
        use = {"i": 0}

        def proj(pbank, pkey, col0, ncol, hkeys):
            slot = use["i"] % NR
            for k in range(8):
                S.op("tensor", lambda e, k=k, slot=slot: e.matmul(pbank[:, :ncol], wr[slot][:, k, :], hT[:, k, col0:col0 + ncol],
                                                                 start=(k == 0), stop=(k == 7)),
                     r=[("wr", slot)] + hkeys, w=[pkey])

        def done_chunk():
            use["i"] += 1
            load_next()

        for q in range(4):
            pieces = [(0, 128, [("hT", 0)])] + [(128 + pc * 512, 512, piece_keys(pc)) for pc in range(4)]
            gc_tiles = []
            for pi, (c0, ncol, hk) in enumerate(pieces):
                pb = ps[pi % 2]
                proj(pb, ("ps", pi % 2), c0, ncol, hk)
                S.op("scalar", lambda e, pb=pb, c0=c0, ncol=ncol: e.activation(gu[:, c0 - 128:c0 - 128 + ncol] if c0 >= 128 else gcs[0][:, :ncol],
                                                                              pb[:, :ncol], AF.Copy),
                     r=[("ps", pi % 2)], w=[(("gu", pi - 1) if pi >= 1 else "gcs0")])
            done_chunk()
            for pi, (c0, ncol, hk) in enumerate(pieces):
                pb = ps[2 + pi % 2]
                proj(pb, ("ps", 2 + pi % 2), c0, ncol, hk)
                gsrc = (gu[:, c0 - 128:c0 - 128 + ncol] if c0 >= 128 else gcs[0][:, :ncol])
                S.op("vector", lambda e, pb=pb, c0=c0, ncol=ncol, gsrc=gsrc: e.tensor_tensor(pT_[:, c0:c0 + ncol], gsrc, pb[:, :ncol], ALU.mult),
                     r=[("ps", 2 + pi % 2), (("gu", pi - 1) if pi >= 1 else "gcs0")], w=[("pT", pi)])
            done_chunk()
            PT = [("pT", pi) for pi in range(5)]
            S.op("vector", lambda e: e.tensor_scalar(pT_[:, 0:128], pT_[:, 0:128], flag[:, 0:1], None, ALU.mult),
                 r=[("pT", 0), "flag"], w=[("pT", 0)])
            S.op("vector", lambda e, q=q: e.tensor_scalar(acc[:], pT_[:, 126:126 + TOK], cw[:, q, 0:1], cw[:, q, 3:4], ALU.mult, ALU.add),
                 r=PT + ["cw"], w=["acc"])
            S.op("vector", lambda e, q=q: e.scalar_tensor_tensor(acc[:], pT_[:, 127:127 + TOK], cw[:, q, 1:2], acc[:], ALU.mult, ALU.add),
                 r=PT + ["cw", "acc"], w=["acc"])
            S.op("vector", lambda e, q=q: e.scalar_tensor_tensor(acc[:], pT_[:, 128:128 + TOK], cw[:, q, 2:3], acc[:], ALU.mult, ALU.add),
                 r=PT + ["cw", "acc"], w=["acc"])
            for pc in range(4):
                pb = ps[pc % 2]
                proj(pb, ("ps", pc % 2), 128 + pc * 512, 512, piece_keys(pc))
                S.op("vector", lambda e, pb=pb, pc=pc, q=q: e.tensor_tensor(ycT[:, q, pc * 512:(pc + 1) * 512], acc[:, pc * 512:(pc + 1) * 512], pb[:], ALU.mult),
                     r=[("ps", pc % 2), "acc"], w=[("ycT", q, pc)])
            done_chunk()

        for g in range(4):
            for pc in range(4):
                pb = ps[pc % 2]
                proj(pb, ("ps", pc % 2), 128 + pc * 512, 512, piece_keys(pc))
                emit_gelu(S, pb[:], [("ps", pc % 2)], gu[:, pc * 512:(pc + 1) * 512], [("gu", pc)], s1[pc % 2], ("s1", pc % 2), 512)
            done_chunk()
            for pc in range(4):
                pm = ps[2 + pc % 2]
                for a in range(4):
                    t = pc * 4 + a
                    S.op("tensor", lambda e, a=a, t=t, g=g, pm=pm: e.matmul(pm[:, a * 128:(a + 1) * 128], vn[:, t, g * 128:(g + 1) * 128], wmT[:, g, :],
                                                                         start=True, stop=True),
                         r=[("vn", t), ("wmT", g)], w=[("ps", 2 + pc % 2)])
                s1t = s1[pc % 2]
                S.op("vector", lambda e, pm=pm, g=g, s1t=s1t: e.tensor_tensor(s1t[:], pm[:], sgub[:, g, :], ALU.add),
                     r=[("ps", 2 + pc % 2), "sgub"], w=[("s1", pc % 2)])
                S.op("vector", lambda e, g=g, pc=pc, s1t=s1t: e.tensor_tensor(ydT[:, g, pc * 512:(pc + 1) * 512], s1t[:], gu[:, pc * 512:(pc + 1) * 512], ALU.mult),
                     r=[("s1", pc % 2), ("gu", pc)], w=[("ydT", g, pc)])

        def lhs_fn(k, t):
            return (ycT[:, k, t * 128:(t + 1) * 128] if k < 4 else ydT[:, k - 4, t * 128:(t + 1) * 128])

        def lhs_keys(k, t):
            return [("ycT", k, t // 4)] if k < 4 else [("ydT", k - 4, t // 4)]
        emit_out_proj_ln(S, sb, ps, lhs_fn, lhs_keys, woS, "wo", gv, MODR, lng, lnb, x_d, y_d, NT, tmp, xin, st6[:, 0:2, :], mv[:, 0, :], rstd[:, 0:1])
        S.emit(nc)
    return nc


def lay_cols_rhs(w):
    return np.ascontiguousarray(w.reshape(8, 128, w.shape[1]).transpose(1, 0, 2))


def mod_inputs(c_row, ada_w_l, ada_b_l, s, lng, lnb):
    sl = slice(3 * s * D, 3 * (s + 1) * D)
    return {
        "cT": np.ascontiguousarray(c_row.reshape(8, 128).T),
        "adaw": lay_adaw(ada_w_l[:, sl]),
        "adab": np.ascontiguousarray(np.broadcast_to(ada_b_l[sl][None, :], (128, 3 * D))),
        "lng": np.ascontiguousarray(np.broadcast_to(lng[None, :], (128, D))),
        "lnb": np.ascontiguousarray(np.broadcast_to(lnb[None, :], (128, D))),
        "ident": np.eye(128, dtype=np.float32),
    }


def cd_inputs(x_sh, x_halo, has_prev, c_row, ada_w_l, ada_b_l, lng, lnb, w_in, w_out, conv_w, conv_b, sln_g, sln_b, sgu_w, sgu_b):
    m = mod_inputs(c_row, ada_w_l, ada_b_l, 1, lng, lnb)
    cw = np.zeros((128, 4, 4), np.float32)
    cw[:, :, 0:3] = conv_w.T.reshape(4, 128, 3).transpose(1, 0, 2)
    cw[:, :, 3] = conv_b.reshape(4, 128).T
    tri = (np.arange(128)[:, None] <= np.arange(128)[None, :]).astype(np.float32)
    m.update({
        "x": np.ascontiguousarray(x_sh), "xh": np.ascontiguousarray(x_halo),
        "flag": np.full((128, 1), 1.0 if has_prev else 0.0, np.float32),
        "win": lay_w_in(w_in[:, :2048]), "wv": lay_cols_rhs(w_in[:, 2048:2560]), "wo": lay_cols_rhs(w_out),
        "cw": cw,
        "sgg": np.ascontiguousarray(np.broadcast_to(sln_g.reshape(1, 512), (128, 512))),
        "sgb": np.ascontiguousarray(np.broadcast_to(sln_b.reshape(1, 512), (128, 512))),
        "wm": np.ascontiguousarray(sgu_w.transpose(2, 0, 1)),
        "msk": tri,
        "sgub": np.ascontiguousarray(np.broadcast_to(sgu_b[None, :, None, :], (128, 4, 4, 128)).reshape(128, 4, 512)),
    })
    return m


class Launch:
    def __init__(self):
        from contextlib import ExitStack
        self.nc = bass.Bass("TRN2", target_bir_lowering=False)
        self.S = Sched()
        self.es = ExitStack()
        self.ps = [self.es.enter_context(self.nc.psum_tensor(f"ps{i}", [128, 512], F32)) for i in range(8)]

    def sb(self, name, shape, dt):
        return self.es.enter_context(self.nc.sbuf_tensor("s_" + name, shape, dt))

    def din(self, name, shape, dt=F32):
        return self.nc.dram_tensor(name, list(shape), dt, kind="ExternalInput").ap()

    def dout(self, name, shape, dt=F32):
        return self.nc.dram_tensor(name, list(shape), dt, kind="ExternalOutput").ap()

    def load(self, name, shape, dt, src, queue="sync", key=None):
        t = self.sb(name, shape, dt)
        self.S.dma(queue, lambda e: e.dma_start(out=t[:], in_=src), name, w=[key or name])
        return t

    def finish(self):
        self.S.emit(self.nc)
        self.es.close()
        return self.nc


def launch_prologue(L, res_w=1.0, want_ln=False, stage=None):
    x_d = L.din("x", [TOK, D])
    cT_d = L.din("cT", [128, 8])
    adaw_d = L.din("adaw", [6, 128, 8, 512])
    adab_d = L.din("adab", [128, 3 * D])
    lng_d = L.din("lng", [128, D])
    lnb_d = L.din("lnb", [128, D])
    ident_d = L.din("ident", [128, 128])
    ident, ones_bf = emit_consts(L.S, L.sb, ident_d)
    sh, sc1, gv, MODR = emit_mod3(L.S, L.sb, L.ps, ones_bf, cT_d, adaw_d, adab_d, res_w, stage=stage)
    lng = lnb = None
    if want_ln:
        lng = L.load("lngS", [128, D], F32, lng_d[:, :], key="lng")
        lnb = L.load("lnbS", [128, D], F32, lnb_d[:, :], key="lnb")
    return dict(x_d=x_d, ident=ident, ones_bf=ones_bf, sh=sh, sc1=sc1, gv=gv, MODR=MODR, lng=lng, lnb=lnb)


def emit_hT_all(L, P, hT, ntiles=NT):
    S = L.S
    tmp = [L.sb(f"tmp{i}", [128, D], F32) for i in range(2)]
    hb = [L.sb(f"hb{i}", [128, D], BF16) for i in range(2)]
    xin = [L.sb(f"xin{i}", [128, D], F32) for i in range(2)]
    for i in range(ntiles):
        xt = xin[i % 2]
        S.dma("sync", lambda e, xt=xt, i=i: e.dma_start(out=xt[:], in_=P["x_d"][i * 128:(i + 1) * 128, :]), f"xin{i % 2}", w=[("xin", i % 2)])
        emit_h_transpose(S, xt[:], ("xin", i % 2), tmp, hb, i % 2, P["sc1"], P["sh"], P["MODR"], P["ident"], L.ps[2 + i % 2],
                         ("ps", 2 + i % 2), (lambda i=i: hT[:, :, i * 128:(i + 1) * 128]), ("hT", i))
    return tmp, hb, xin


def emit_featmajor_proj(L, w_d, nchunks, hT, dst, dkey, ps_ids=(0, 1), nring=2, name="wf", mc=128):
    S = L.S
    wr = [L.sb(f"{name}{i}", [128, 8, mc], BF16) for i in range(nring)]
    for c in range(min(nring, nchunks)):
        S.dma("gpsimd", lambda e, c=c: e.dma_start(out=wr[c % nring][:], in_=w_d[c]), f"{name}{c % nring}", w=[(name, c % nring)])
    for c in range(nchunks):
        slot = c % nring
        for pc in range(TOK // 512):
            pid = ps_ids[pc % len(ps_ids)]
            pb = L.ps[pid]
            for k in range(8):
                S.op("tensor", lambda e, k=k, slot=slot, pc=pc, pb=pb: e.matmul(pb[0:mc, :], wr[slot][:, k, :], hT[:, k, pc * 512:(pc + 1) * 512],
                                                                             start=(k == 0), stop=(k == 7)),
                     r=[(name, slot)] + [("hT", pc * 4 + a) for a in range(4)], w=[("ps", pid)])
            S.op("scalar", lambda e, c=c, pc=pc, pb=pb: e.activation(dst[:, c, pc * 512:(pc + 1) * 512], pb[0:mc, :], AF.Copy),
                 r=[("ps", pid)], w=[(dkey, c, pc)])
        if c + nring < nchunks:
            c2 = c + nring
            S.dma("gpsimd", lambda e, c2=c2: e.dma_start(out=wr[c2 % nring][:], in_=w_d[c2]), f"{name}{c2 % nring}", w=[(name, c2 % nring)])


def emit_mlstm_chunk_prep(L, t, hT, W, C, pools):
    S = L.S
    ps = L.ps
    hk = [("hT", t)]
    col = slice(t * 128, (t + 1) * 128)
    gsb, sp, e1, tsum, bs, thr, expF, mk_sb, Vp, tC = (pools[n] for n in ("gsb", "sp", "e1", "tsum", "bs", "thr", "expF", "mk_sb", "Vp", "tC"))
    for k in range(8):
        S.op("tensor", lambda e, k=k: e.matmul(ps[0][:, 0:8], hT[:, k, col], W["wgt"][:, k, :], start=(k == 0), stop=(k == 7)),
             r=hk + ["wgt"], w=[("ps", 0)])
    S.op("vector", lambda e: e.tensor_tensor(gsb[:], ps[0][:, 0:8], W["bgate"][:], ALU.add), r=[("ps", 0), "bgate"], w=["gsb"])
    S.op("scalar", lambda e: e.activation(e1[:], gsb[:, 4:8], AF.Exp, scale=-1.0), r=["gsb"], w=["e1"])
    S.op("scalar", lambda e: e.activation(sp[:], e1[:], AF.Ln, bias=1.0), r=["e1"], w=["sp"])
    S.op("tensor", lambda e: e.matmul(ps[0][:, 16:20], W["tri"][:], sp[:], start=True, stop=True), r=["tri", "sp"], w=[("ps", 0)])
    S.op("tensor", lambda e: e.matmul(ps[0][:, 24:28], W["onesf"][:], sp[:], start=True, stop=True), r=["onesf", "sp"], w=[("ps", 0)])
    S.op("vector", lambda e: e.tensor_tensor(tsum[:], gsb[:, 0:4], ps[0][:, 16:20], ALU.add), r=["gsb", ("ps", 0)], w=["tsum"])
    S.op("scalar", lambda e: e.activation(bs[:], tsum[:], AF.Exp, bias=float(-np.log(8.0))), r=["tsum"], w=["bs"])
    S.op("scalar", lambda e: e.activation(thr[:], ps[0][:, 16:20], AF.Exp), r=[("ps", 0)], w=["thr"])
    S.op("scalar", lambda e: e.activation(expF[:], ps[0][0:64, 24:28], AF.Exp, scale=-1.0), r=[("ps", 0)], w=["expF"])
    for k in range(8):
        S.op("tensor", lambda e, k=k: e.matmul(ps[1][:], hT[:, k, col], W["wmv"][:, k, :], start=(k == 0), stop=(k == 7)),
             r=hk + ["wmv"], w=[("ps", 1)])
    for h in range(4):
        S.op("vector", lambda e, h=h: e.tensor_scalar(Vp[:, h, 0:128], ps[1][:, h * 128:(h + 1) * 128], bs[:, h:h + 1], None, ALU.mult),
             r=[("ps", 1), "bs"], w=[("Vp", h)])
    S.op("vector", lambda e: e.tensor_copy(Vp[:, :, 128], bs[:]), r=["bs"] + [("Vp", h) for h in range(4)], w=[("Vp", h) for h in range(4)])
    for k in range(8):
        S.op("tensor", lambda e, k=k: e.matmul(ps[0][:, 256:512], hT[:, k, col], W["wmk"][:, k, :], start=(k == 0), stop=(k == 7)),
             r=hk + ["wmk"], w=[("ps", 0)])
    S.op("scalar", lambda e: e.activation(mk_sb[:], ps[0][:, 256:512], AF.Copy), r=[("ps", 0)], w=["mk_sb"])


def emit_mlstm_state_update(L, C, pools):
    S = L.S
    ps = L.ps
    mk_sb, Vp, expF, tC = pools["mk_sb"], pools["Vp"], pools["expF"], pools["tC"]
    for h in range(4):
        pb = ps[6 + h // 2]
        S.op("tensor", lambda e, h=h, pb=pb: e.matmul(pb[0:64, (h % 2) * 129:(h % 2) * 129 + 129], mk_sb[:, h * 64:(h + 1) * 64], Vp[:, h, :],
                                                     start=True, stop=True),
             r=["mk_sb", ("Vp", h)], w=[("ps", 6 + h // 2)])
    for h in range(4):
        pb = ps[6 + h // 2]
        S.op("vector", lambda e, h=h, pb=pb: e.tensor_tensor(tC[:, h, :], C[:, h, :], pb[0:64, (h % 2) * 129:(h % 2) * 129 + 129], ALU.add),
             r=[("ps", 6 + h // 2), ("C", h)], w=[("tC", h)])
        S.op("vector", lambda e, h=h: e.tensor_scalar(C[:, h, :], tC[:, h, :], expF[:, h:h + 1], None, ALU.mult),
             r=[("tC", h), "expF"], w=[("C", h)])


def mlstm_pools(L):
    p = {}
    p["gsb"] = L.sb("gsb", [128, 8], F32)
    p["sp"] = L.sb("sp", [128, 4], F32)
    p["e1"] = L.sb("e1", [128, 4], F32)
    p["tsum"] = L.sb("tsum", [128, 4], F32)
    p["bs"] = L.sb("bs", [128, 4], F32)
    p["thr"] = L.sb("thr", [128, 4], F32)
    p["expF"] = L.sb("expF", [64, 4], F32)
    p["mk_sb"] = L.sb("mk_sb", [128, 256], BF16)
    p["Vp"] = L.sb("Vp", [128, 4, 129], BF16)
    p["tC"] = L.sb("tC", [64, 4, 129], F32)
    return p


def mlstm_weights(L):
    W = {}
    wmk_d = L.din("wmk", [128, 8, 256]); wmv_d = L.din("wmv", [128, 8, 512]); wgt_d = L.din("wgt", [128, 8, 8])
    W["wmk"] = L.load("wmk", [128, 8, 256], BF16, wmk_d, "gpsimd")
    W["wmv"] = L.load("wmv", [128, 8, 512], BF16, wmv_d, "gpsimd")
    W["wgt"] = L.load("wgt", [128, 8, 8], BF16, wgt_d, "gpsimd")
    W["bgate"] = L.load("bgate", [128, 8], F32, L.din("bgate", [128, 8]))
    W["tri"] = L.load("tri", [128, 128], F32, L.din("tri", [128, 128]))
    W["onesf"] = L.load("onesf", [128, 128], F32, L.din("onesf", [128, 128]))
    return W


def build_ab_producer():
    L = Launch()
    S = L.S
    P = launch_prologue(L)
    wk_d = L.din("wk", [8, 128, 8, 64])
    wav_d = L.din("wav", [128, 8, 512])
    kT_d = L.dout("kT", [64, 8, TOK], BF16)
    vx_d = L.dout("vx", [TOK, 520], BF16)
    cseg_d = L.dout("cseg", [64, 4 * 129])
    sseg_d = L.dout("sseg", [64, 4])
    hT = L.sb("hT", [128, 8, TOK], BF16)
    emit_hT_all(L, P, hT)
    kTs = L.sb("kTs", [64, 8, TOK], BF16)
    emit_featmajor_proj(L, wk_d, 8, hT, kTs, "kTs", ps_ids=(4, 5), mc=64)
    S.dma("sync", lambda e: e.dma_start(out=kT_d, in_=kTs[:]), "kTout", r=[("kTs", c, pc) for c in range(8) for pc in range(4)])
    wav = L.load("wav", [128, 8, 512], BF16, wav_d, "gpsimd")
    W = mlstm_weights(L)
    pools = mlstm_pools(L)
    C = L.sb("Cst", [64, 4, 129], F32)
    ssum = L.sb("ssum", [64, 4], F32)
    S.op("vector", lambda e: e.memset(C[:], 0.0), w=[("C", h) for h in range(4)])
    S.op("vector", lambda e: e.memset(ssum[:], 0.0), w=["ssum"])
    vxs = [L.sb(f"vxs{i}", [128, 8, 65], BF16) for i in range(2)]
    for i in range(2):
        S.op("vector", lambda e, i=i: e.memset(vxs[i][:], 1.0), w=[("vxs", i)])
    lnexp = L.sb("lnexp", [64, 4], F32)
    for t in range(NT):
        col = slice(t * 128, (t + 1) * 128)
        for k in range(8):
            S.op("tensor", lambda e, k=k, col=col, t=t: e.matmul(L.ps[4 + t % 2][:], hT[:, k, col], wav[:, k, :], start=(k == 0), stop=(k == 7)),
                 r=[("hT", t), "wav"], w=[("ps", 4 + t % 2)])
        vt = vxs[t % 2]
        S.op("scalar", lambda e, vt=vt, t=t: e.activation(vt[:, :, 0:64], L.ps[4 + t % 2][:].rearrange("p (h c) -> p h c", h=8), AF.Copy),
             r=[("ps", 4 + t % 2)], w=[("vxs", t % 2)])
        S.dma("sync", lambda e, vt=vt, t=t: e.dma_start(out=vx_d[t * 128:(t + 1) * 128, :], in_=vt[:].rearrange("p h c -> p (h c)")),
              f"vxo{t % 2}", r=[("vxs", t % 2)])
        emit_mlstm_chunk_prep(L, t, hT, W, C, pools)
        emit_mlstm_state_update(L, C, pools)
        S.op("scalar", lambda e: e.activation(lnexp[:], pools["expF"][:], AF.Ln), r=["expF"], w=["lnexp"])
        S.op("vector", lambda e: e.tensor_tensor(ssum[:], ssum[:], lnexp[:], ALU.subtract), r=["lnexp", "ssum"], w=["ssum"])
    S.dma("sync", lambda e: e.dma_start(out=cseg_d, in_=C[:].rearrange("p a b -> p (a b)")), "cout", r=[("C", h) for h in range(4)])
    S.dma("sync", lambda e: e.dma_start(out=sseg_d, in_=ssum[:]), "sout", r=["ssum"])
    S.final_waits("sync", ["D:kTout", "D:vxo0", "D:vxo1", "D:cout", "D:sout"])
    return L.finish()


def build_ab_mlstm():
    L = Launch()
    S = L.S
    ps = L.ps
    P = launch_prologue(L)
    wq_d = L.din("wq", [4, 128, 8, 64])
    wkf_d = L.din("wkf", [4, 128, 8, 64])
    wmo_d = L.din("wmo", [128, 8, 512])
    cprev_d = L.din("cprev", [64, 3, 4 * 129])
    sprev_d = L.din("sprev", [64, 3, 4])
    msk_d = L.din("cmask", [128, 512])
    haT_d = L.dout("haT", [128, 4, TOK], BF16)
    hT = L.sb("hT", [128, 8, TOK], BF16)
    emit_hT_all(L, P, hT)
    mqT = L.sb("mqT", [64, 4, TOK], BF16)
    mkT = L.sb("mkT", [64, 4, TOK], BF16)
    emit_featmajor_proj(L, wq_d, 4, hT, mqT, "mqT", ps_ids=(4, 5), name="wfq", mc=64)
    emit_featmajor_proj(L, wkf_d, 4, hT, mkT, "mkT", ps_ids=(4, 5), name="wfk", mc=64)
    wmo = L.load("wmo", [128, 8, 512], BF16, wmo_d, "gpsimd")
    W = mlstm_weights(L)
    pools = mlstm_pools(L)
    cmask = L.load("cmask", [128, 512], F32, msk_d)
    cprev = L.load("cprev", [64, 3, 516], F32, cprev_d)
    sprev = L.load("sprev", [64, 3, 4], F32, sprev_d)
    C = L.sb("Cst", [64, 4, 129], F32)
    Cb = L.sb("Cb", [64, 4, 129], BF16)
    haT = L.sb("haT", [128, 4, TOK], BF16)
    a12 = L.sb("a12", [64, 2, 4], F32)
    e12 = L.sb("e12", [64, 2, 4], F32)
    S.op("vector", lambda e: e.tensor_copy(a12[:, 0, :], sprev[:, 0, :]), r=["sprev"], w=["a12"])
    S.op("vector", lambda e: e.tensor_tensor(a12[:, 1, :], sprev[:, 0, :], sprev[:, 1, :], ALU.add), r=["sprev", "a12"], w=["a12"])
    S.op("scalar", lambda e: e.activation(e12[:], a12[:], AF.Exp, scale=-1.0), r=["a12"], w=["e12"])
    CK = [("C", h) for h in range(4)]
    S.op("vector", lambda e: e.tensor_copy(C[:], cprev[:, 0, :].rearrange("p (a b) -> p a b", a=4)), r=["cprev"], w=CK)
    for sl in (1, 2):
        for h in range(4):
            S.op("vector", lambda e, sl=sl, h=h: e.scalar_tensor_tensor(
                C[:, h, :], cprev[:, sl, h * 129:(h + 1) * 129], e12[:, sl - 1, h:h + 1], C[:, h, :], ALU.mult, ALU.add),
                r=["cprev", "e12"] + CK, w=CK)
    SM = L.sb("SM", [128, 512], BF16)
    sig = L.sb("sig", [128, 512], F32)
    hab = L.sb("hab", [128, 512], BF16)
    den = L.sb("den", [128, 4], F32)
    nden = L.sb("nden", [128, 4], F32)
    for t in range(NT):
        col = slice(t * 128, (t + 1) * 128)
        emit_mlstm_chunk_prep(L, t, hT, W, C, pools)
        S.op("vector", lambda e: e.tensor_copy(Cb[:], C[:]), r=CK, w=["Cb"])
        emit_mlstm_state_update(L, C, pools)
        for h in range(4):
            S.op("tensor", lambda e, h=h, col=col: e.matmul(ps[4][:, h * 128:(h + 1) * 128], mkT[:, h, col], mqT[:, h, col],
                                                         start=True, stop=True),
                 r=[("mkT", h, t // 4), ("mqT", h, t // 4)], w=[("ps", 4)])
        S.op("vector", lambda e: e.tensor_tensor(SM[:], ps[4][:], cmask[:], ALU.mult), r=[("ps", 4), "cmask"], w=["SM"])
        for h in range(4):
            pair, hh, pr = h // 2, h % 2, slice((h % 2) * 64, (h % 2) * 64 + 64)
            ob = ps[5][:, hh * 129:hh * 129 + 129] if pair == 0 else ps[3][:, hh * 129:hh * 129 + 129]
            okey = ("ps", 5) if pair == 0 else ("ps", 3)
            S.op("tensor", lambda e, h=h, ob=ob: e.matmul(ob, SM[:, h * 128:(h + 1) * 128], pools["Vp"][:, h, :], start=True, stop=False),
                 r=["SM", ("Vp", h)], w=[okey])
            S.op("tensor", lambda e, h=h, ob=ob, col=col: e.matmul(ob, mqT[:, h, col], Cb[:, h, :], start=False, stop=True),
                 r=[("mqT", h, t // 4), "Cb"], w=[okey])
        for k in range(8):
            S.op("tensor", lambda e, k=k, col=col: e.matmul(ps[2][:], hT[:, k, col], wmo[:, k, :], start=(k == 0), stop=(k == 7)),
                 r=[("hT", t), "wmo"], w=[("ps", 2)])
        S.op("scalar", lambda e: e.activation(sig[:], ps[2][:], AF.Sigmoid), r=[("ps", 2)], w=["sig"])
        for pair in range(2):
            src = ps[5] if pair == 0 else ps[3]
            okey = ("ps", 5) if pair == 0 else ("ps", 3)
            S.op("vector", lambda e, pair=pair, src=src: e.tensor_copy(den[:, pair * 2:pair * 2 + 2], src[:, 128:258:129]), r=[okey], w=["den"])
        S.op("vector", lambda e: e.tensor_scalar(nden[:], den[:], -1.0, None, ALU.mult), r=["den"], w=["nden"])
        S.op("vector", lambda e: e.tensor_tensor(den[:], den[:], nden[:], ALU.max), r=["den", "nden"], w=["den"])
        S.op("vector", lambda e: e.tensor_tensor(den[:], den[:], pools["thr"][:], ALU.max), r=["den", "thr"], w=["den"])
        S.op("vector", lambda e: e.reciprocal(den[:], den[:]), r=["den"], w=["den"])
        for h in range(4):
            pair, hh = h // 2, h % 2
            src = ps[5] if pair == 0 else ps[3]
            okey = ("ps", 5) if pair == 0 else ("ps", 3)
            S.op("vector", lambda e, h=h, hh=hh, src=src: e.scalar_tensor_tensor(
                hab[:, h * 128:(h + 1) * 128], src[:, hh * 129:hh * 129 + 128], den[:, h:h + 1], sig[:, h * 128:(h + 1) * 128], ALU.mult, ALU.mult),
                r=[okey, "den", "sig"], w=["hab"])
        pTb = ps[1][:].bitcast(BF16)
        for h in range(4):
            S.op("tensor", lambda e, h=h, pTb=pTb: e.transpose(pTb[:, h * 128:(h + 1) * 128], hab[:, h * 128:(h + 1) * 128], P["ident"][:]),
                 r=["hab", "ident"], w=[("ps", 1)])
        S.op("scalar", lambda e, pTb=pTb, col=col: e.activation(haT[:, :, col], pTb[:, 0:512].rearrange("p (h t) -> p h t", h=4), AF.Copy),
             r=[("ps", 1)], w=[("haT", t)])
    S.dma("sync", lambda e: e.dma_start(out=haT_d, in_=haT[:]), "haout", r=[("haT", t) for t in range(NT)])
    S.final_waits("sync", ["D:haout"])
    return L.finish()


DIL = (1, 4, 16)


def build_ab_attn():
    L = Launch()
    S = L.S
    ps = L.ps
    vb = [L.sb(f"vb{i}", [128, 3, 32 * 65], BF16) for i in range(2)]
    stage = [(vb[i][:].rearrange("p a b -> p (a b)")[:, 0:4096].rearrange("p (k c) -> p k c", k=8), ("vb", i)) for i in range(2)]
    P = launch_prologue(L, stage=stage)
    wq_d = L.din("wqa", [8, 128, 8, 64])
    akT_d = L.din("akT", [64, 8, 2 * TOK], BF16)
    vb_d = L.din("vb", [8, 3, 128, 32 * 65], BF16)
    bias_d = L.din("biasm", [8, 128, 9 * 128])
    onesr_d = L.din("onesr", [128, 64])
    hbT_d = L.dout("hbT", [64, 8, TOK], BF16)
    hT = L.sb("hT", [128, 8, TOK], BF16)
    emit_hT_all(L, P, hT)
    aqT = L.sb("aqT", [64, 8, TOK], BF16)
    emit_featmajor_proj(L, wq_d, 8, hT, aqT, "aqT", ps_ids=(4, 5), name="wfq", mc=64)
    akTb = [L.sb(f"akT{i}", [64, 2 * TOK], BF16) for i in range(2)]
    onesr = L.load("onesr", [128, 64], F32, onesr_d)
    hbT = L.sb("hbT", [64, 8, TOK], BF16)
    biasf = L.sb("biasf", [128, 9 * 128], F32)
    E = [L.sb(f"E{i}", [128, 9, 128], BF16) for i in range(2)]
    NPB = 3
    Pe = [L.sb(f"Pe{i}", [128, 512], BF16) for i in range(NPB)]
    PT = [L.sb(f"PT{i}", [128, 512], BF16) for i in range(NPB)]
    lrow = L.sb("lrow", [128, 512], F32)
    bcs = L.sb("bcs", [64, 512], F32)
    AQ = [("aqT", c, pc) for c in range(8) for pc in range(4)]
    l0 = L.sb("l0", [1, 512], F32)
    OWN = TOK
    cnt = {"s": 0}
    for h in range(8):
        vbt, Et, akT = vb[h % 2], E[h % 2], akTb[h % 2]
        S.dma("sync", lambda e, akT=akT, h=h: e.dma_start(out=akT[:], in_=akT_d[:, h, :]), f"akT{h % 2}", w=[("akT", h % 2)])
        for br in range(3):
            S.dma("sync", lambda e, br=br, vbt=vbt, h=h: e.dma_start(out=vbt[:, br, :], in_=vb_d[h, br]), f"vb{h % 2}", w=[("vb", h % 2)])
        S.dma("sync", lambda e, h=h: e.dma_start(out=biasf[:], in_=bias_d[h]), "biasf", w=["biasf"])
        S.op("scalar", lambda e, Et=Et: e.activation(Et[:].rearrange("p a b -> p (a b)"), biasf[:], AF.Exp), r=["biasf"], w=[("E", h % 2)])

        def score_group(items):
            i = cnt["s"]
            cnt["s"] += 1
            bank = 4 + i % 2
            for j, (kc, qc, ek) in enumerate(items):
                S.op("tensor", lambda e, j=j, kc=kc, qc=qc, bank=bank, h=h, akT=akT: e.matmul(ps[bank][:, j * 128:(j + 1) * 128], akT[:, kc], aqT[:, h, qc],
                                                                             start=True, stop=True),
                     r=[("akT", h % 2)] + AQ, w=[("ps", bank)])
            pe, pt = Pe[i % NPB], PT[i % NPB]
            n = len(items) * 128
            S.op("scalar", lambda e, pe=pe, bank=bank, n=n: e.activation(pe[:, :n], ps[bank][:, :n], AF.Exp, scale=0.125), r=[("ps", bank)], w=[("Pe", i % NPB)])
            for j, (kc, qc, ek) in enumerate(items):
                S.op("vector", lambda e, j=j, ek=ek, pe=pe, pt=pt, Et=Et: e.tensor_tensor(pt[:, j * 128:(j + 1) * 128], pe[:, j * 128:(j + 1) * 128], Et[:, ek, :], ALU.mult),
                     r=[("Pe", i % NPB), ("E", h % 2)], w=[("PT", i % NPB, j)])
            return pt, ("PT", i % NPB)

        def pv(pt, ptkey, j0, ncols, blk_br, blk, sp, ocols, first, last):
            S.op("tensor", lambda e, vbt=vbt: e.matmul(ps[sp][0:65, ocols], vbt[:, blk_br, blk * 65:(blk + 1) * 65], pt[:, j0:j0 + ncols],
                                              start=first, stop=last, skip_group_check=True),
                 r=[ptkey + (j0 // 128,), ("vb", h % 2)], w=[("ps", sp)])

        for sp in range(4):
            for kind in (0, 1):
                items = []
                for a in range(4):
                    qb = sp * 4 + a
                    qc = slice(qb * 128, (qb + 1) * 128)
                    if kind == 0:
                        kc, ek = slice(OWN + qb * 128, OWN + (qb + 1) * 128), 0
                    else:
                        kc = slice(OWN + (qb - 1) * 128, OWN + qb * 128)
                        ek = 1 if qb >= 1 else 2
                    items.append((kc, qc, ek))
                pt, ptk = score_group(items)
                for a in range(4):
                    qb = sp * 4 + a
                    blk = 16 + qb if kind == 0 else 16 + qb - 1
                    pv(pt, ptk, a * 128, 128, 0, blk, sp, slice(a * 128, (a + 1) * 128), first=(kind == 0 and a == 0), last=False)
        for sp in range(4):
            for kind in (0, 1):
                items = []
                for r in range(4):
                    qc = slice(512 * sp + r, 512 * sp + 512, 4)
                    ksp = sp if kind == 0 else sp - 1
                    kc = slice(OWN + 512 * ksp + r, OWN + 512 * ksp + 512, 4)
                    ek = 3 if kind == 0 else (4 if sp >= 1 else 5)
                    items.append((kc, qc, ek))
                pt, ptk = score_group(items)
                for r in range(4):
                    blk = (4 + sp) * 4 + r if kind == 0 else (4 + sp - 1) * 4 + r
                    pv(pt, ptk, r * 128, 128, 1, blk, sp, slice(r, 512, 4), first=False, last=False)
        for rg in range(4):
            for kind in (0, 1):
                items = []
                for a in range(4):
                    r = rg * 4 + a
                    qc = slice(r, TOK, 16)
                    kc = slice(OWN + r, OWN + TOK, 16) if kind == 0 else slice(r, TOK, 16)
                    items.append((kc, qc, 6 if kind == 0 else 8))
                pt, ptk = score_group(items)
                for a in range(4):
                    r = rg * 4 + a
                    blk = 16 + r if kind == 0 else r
                    for sp in range(4):
                        pv(pt, ptk, a * 128 + 32 * sp, 32, 2, blk, sp, slice(r, 512, 16), first=False, last=(kind == 1))
        for sp in range(4):
            S.op("scalar", lambda e, sp=sp: e.activation(lrow[64:65, :], ps[sp][64:65, :], AF.Copy), r=[("ps", sp)], w=["lrow"])
            S.dma("sync", lambda e: e.dma_start(out=l0[:], in_=lrow[64:65, :]), "l0", r=["lrow"], w=["l0"])
            S.op("vector", lambda e: e.reciprocal(l0[:], l0[:]), r=["l0"], w=["l0"])
            bank = 6 + sp % 2
            S.op("tensor", lambda e, bank=bank: e.matmul(ps[bank][0:64, :], onesr[0:1, :], l0[:], start=True, stop=True),
                 r=["l0", "onesr"], w=[("ps", bank)])
            S.op("scalar", lambda e, bank=bank: e.activation(bcs[:], ps[bank][0:64, :], AF.Copy), r=[("ps", bank)], w=["bcs"])
            S.op("vector", lambda e, sp=sp, h=h: e.tensor_tensor(hbT[:, h, sp * 512:(sp + 1) * 512], ps[sp][0:64, :], bcs[:], ALU.mult),
                 r=[("ps", sp), "bcs"], w=[("hbT", h)])
    S.dma("sync", lambda e: e.dma_start(out=hbT_d, in_=hbT[:]), "hbout", r=[("hbT", h) for h in range(8)])
    S.final_waits("sync", ["D:hbout"])
    return L.finish()


def build_ab_out():
    L = Launch()
    S = L.S
    ps = L.ps
    P = launch_prologue(L, want_ln=True)
    haT_d = L.din("haT", [128, 4, TOK], BF16)
    hbT_d = L.din("hbT", [64, 8, TOK], BF16)
    woa_d = L.din("woa", [128, 4, D])
    wob_d = L.din("wob", [64, 8, D])
    y_d = L.dout("y", [TOK, D])
    haT = L.load("haT", [128, 4, TOK], BF16, haT_d)
    hbT = L.load("hbT", [64, 8, TOK], BF16, hbT_d)
    woa = L.load("woa", [128, 4, D], BF16, woa_d, "gpsimd")
    wob = L.load("wob", [64, 8, D], BF16, wob_d, "gpsimd")
    tmp = [L.sb(f"tmp{i}", [128, D], F32) for i in range(2)]
    xin = [L.sb(f"xin{i}", [128, D], F32) for i in range(2)]
    st6 = L.sb("st6", [128, 2, 6], F32)
    mv = L.sb("mv", [128, 2], F32)
    rstd = L.sb("rstd", [128, 1], F32)
    for t in range(NT):
        xt = xin[t % 2]
        col = slice(t * 128, (t + 1) * 128)
        S.dma("sync", lambda e, t=t, xt=xt: e.dma_start(out=xt[:], in_=P["x_d"][t * 128:(t + 1) * 128, :]), f"xin{t % 2}", w=[("xin", t % 2)])
        pa = (t % 2) * 2
        for dh in range(2):
            dc = slice(dh * 512, (dh + 1) * 512)
            for k in range(4):
                S.op("tensor", lambda e, k=k, dc=dc, col=col, pa=pa, dh=dh: e.matmul(ps[pa + dh][:], haT[:, k, col], woa[:, k, dc], start=(k == 0), stop=False),
                     r=["haT", "woa"], w=[("ps", pa + dh)])
            for k in range(8):
                S.op("tensor", lambda e, k=k, dc=dc, col=col, pa=pa, dh=dh: e.matmul(ps[pa + dh][:], hbT[:, k, col], wob[:, k, dc], start=False, stop=(k == 7)),
                     r=["hbT", "wob"], w=[("ps", pa + dh)])
        tm = tmp[t % 2]
        for dh in range(2):
            S.op("vector", lambda e, tm=tm, dh=dh, pa=pa: e.tensor_tensor(
                tm[:, dh * 512:(dh + 1) * 512], ps[pa + dh][:], P["gv"][:, dh * 512:(dh + 1) * 512], ALU.mult),
                r=[("ps", pa + dh)] + P["MODR"], w=[("tmp", t % 2)])
        S.op("vector", lambda e, tm=tm, xt=xt: e.scalar_tensor_tensor(xt[:], xt[:], ALPHA, tm[:], ALU.mult, ALU.add),
             r=[("tmp", t % 2), ("xin", t % 2)], w=[("xin", t % 2)])
        emit_ln(S, xt[:], ("xin", t % 2), st6, mv, rstd, P["lng"], P["lnb"])
        S.dma("sync", lambda e, t=t, xt=xt: e.dma_start(out=y_d[t * 128:(t + 1) * 128, :], in_=xt[:]), f"yo{t % 2}", r=[("xin", t % 2)])
    S.final_waits("sync", ["D:yo0", "D:yo1"])
    return L.finish()


_PROGS = {}


def _prog(name, builder):
    if name not in _PROGS:
        _PROGS[name] = builder()
    return _PROGS[name]


def _run(name, builder, in_maps):
    nc = _prog(name, builder)
    res = run_bass_kernel_spmd(nc, in_maps, core_ids=list(range(NCORES)))
    return res.results


def _rel_bucket(dist):
    max_exact = 16
    d = np.maximum(dist, 0)
    large = max_exact + (np.log(np.maximum(d, 1).astype(np.float32) / np.float32(max_exact))
                         / np.float32(np.log(2048 / max_exact)) * np.float32(32 - max_exact)).astype(np.int32)
    large = np.minimum(large, 31)
    return np.where(d < max_exact, d, large)


def _bias_tables(rel_bias, has_prev):
    NEG = np.float32(-30000.0)
    ik = np.arange(128)[:, None]
    iq = np.arange(128)[None, :]
    out = np.full((8, 128, 9, 128), NEG, np.float32)
    for bi, d in enumerate(DIL):
        m_same = iq - ik
        m_prev = iq + 128 - ik
        b_same = rel_bias[_rel_bucket(m_same * d)]
        b_prev = rel_bias[_rel_bucket(m_prev * d)]
        for h in range(8):
            out[h, :, bi * 3 + 0, :] = np.where(m_same >= 0, b_same[:, :, h], NEG)
            pv = np.where(m_prev <= 128, b_prev[:, :, h], NEG)
            out[h, :, bi * 3 + 1, :] = pv
            out[h, :, bi * 3 + 2, :] = pv if has_prev else NEG
    return out.reshape(8, 128, 9 * 128)


def _vb_layout(V):
    V4 = V.reshape(4096, 8, 65)
    o = np.empty((8, 3, 128, 32, 65), V.dtype)
    o[:, 0] = V4.reshape(32, 128, 8, 65).transpose(2, 1, 0, 3)
    o[:, 1] = V4.reshape(8, 128, 4, 8, 65).transpose(3, 1, 0, 2, 4).reshape(8, 128, 32, 65)
    o[:, 2] = V4.reshape(2, 128, 16, 8, 65).transpose(3, 1, 0, 2, 4).reshape(8, 128, 32, 65)
    return np.ascontiguousarray(o.reshape(8, 3, 128, 32 * 65))


def kernel(x, c, rel_bias, ada_w, ada_b, ln_g, ln_b, ffn_w_gate, ffn_w_up, ffn_w_down,
           ab_w_in, ab_w_out, ab_b_igate, ab_b_fgate,
           cd_w_in, cd_w_out, cd_conv_w, cd_conv_b, cd_sgu_ln_g, cd_sgu_ln_b, cd_sgu_w, cd_sgu_b, _debug=None):
    f32 = np.float32
    A = lambda a: np.asarray(a, dtype=f32)
    x, c, rel_bias, ada_w, ada_b, ln_g, ln_b = map(A, (x, c, rel_bias, ada_w, ada_b, ln_g, ln_b))
    ffn_w_gate, ffn_w_up, ffn_w_down = map(A, (ffn_w_gate, ffn_w_up, ffn_w_down))
    ab_w_in, ab_w_out, ab_b_igate, ab_b_fgate = map(A, (ab_w_in, ab_w_out, ab_b_igate, ab_b_fgate))
    cd_w_in, cd_w_out, cd_conv_w, cd_conv_b = map(A, (cd_w_in, cd_w_out, cd_conv_w, cd_conv_b))
    cd_sgu_ln_g, cd_sgu_ln_b, cd_sgu_w, cd_sgu_b = map(A, (cd_sgu_ln_g, cd_sgu_ln_b, cd_sgu_w, cd_sgu_b))
    cores = [(r // 4, r % 4) for r in range(NCORES)]
    xs = [x[b, j * TOK:(j + 1) * TOK] for (b, j) in cores]

    def ffn_stage(xs, layer, slot):
        s = 0 if slot == 0 else 2
        wg, wu, wd = lay_w_in(ffn_w_gate[layer, slot]), lay_w_in(ffn_w_up[layer, slot]), np.ascontiguousarray(ffn_w_down[layer, slot].reshape(NCH, 128, D))
        maps = []
        for r, (b, j) in enumerate(cores):
            m = mod_inputs(c[b], ada_w[layer], ada_b[layer], s, ln_g[layer, s], ln_b[layer, s])
            m.update({"x": np.ascontiguousarray(xs[r]), "wg": wg, "wu": wu, "wd": wd})
            maps.append(m)
        res = _run("ffn", build_ffn, maps)
        return [res[r]["y"] for r in range(NCORES)]

    def dbg(name, xs):
        if _debug is not None:
            _debug[name] = np.stack(xs).reshape(2, SEQ, D).copy()

    xs = ffn_stage(xs, 0, 0)
    dbg("l0s0", xs)
    w = ab_w_in[0]
    tri = (np.arange(128)[:, None] <= np.arange(128)[None, :]).astype(f32)
    common_m = {
        "wmk": lay_cols_rhs(w[:, 256:512]), "wmv": lay_cols_rhs(w[:, 512:1024]), "wgt": lay_cols_rhs(w[:, 1536:1544]),
        "bgate": np.ascontiguousarray(np.broadcast_to(np.concatenate([ab_b_igate[0], ab_b_fgate[0]])[None, :], (128, 8))),
        "tri": tri, "onesf": np.ones((128, 128), f32),
    }
    mods1 = [mod_inputs(c[b], ada_w[0], ada_b[0], 1, ln_g[0, 1], ln_b[0, 1]) for (b, j) in cores]
    maps = []
    for r in range(NCORES):
        m = dict(mods1[r]); m.update(common_m)
        m.update({"x": np.ascontiguousarray(xs[r]), "wk": lay_w_in(w[:, 2056:2568], 64), "wav": lay_cols_rhs(w[:, 2568:3080])})
        maps.append(m)
    prod = _run("abp", build_ab_producer, maps)
    if _debug is not None:
        _debug["prod"] = prod
    maps = []
    cmask = np.tile((np.arange(128)[None, :] >= np.arange(128)[:, None]).astype(f32), (1, 4))
    for r, (b, j) in enumerate(cores):
        cprev = np.zeros((64, 3, 516), f32)
        sprev = np.zeros((64, 3, 4), f32)
        for k in range(3):
            if j - 1 - k >= 0:
                cprev[:, k] = prod[r - 1 - k]["cseg"]
                sprev[:, k] = prod[r - 1 - k]["sseg"]
        m = dict(mods1[r]); m.update(common_m)
        m.update({"x": np.ascontiguousarray(xs[r]), "wq": lay_w_in(w[:, 0:256], 64), "wkf": lay_w_in(w[:, 256:512], 64),
                  "wmo": lay_cols_rhs(w[:, 1024:1536]), "cprev": cprev, "sprev": sprev, "cmask": cmask})
        maps.append(m)
    ml = _run("abm", build_ab_mlstm, maps)
    maps = []
    for r, (b, j) in enumerate(cores):
        own_k, own_v = prod[r]["kT"], prod[r]["vx"]
        if j > 0:
            pk, pvx = prod[r - 1]["kT"], prod[r - 1]["vx"]
        else:
            pk, pvx = np.zeros_like(own_k), np.zeros_like(own_v)
        m = dict(mods1[r])
        m.update({"x": np.ascontiguousarray(xs[r]), "wqa": lay_w_in(w[:, 1544:2056], 64),
                  "akT": np.ascontiguousarray(np.concatenate([pk, own_k], axis=2)),
                  "vb": _vb_layout(np.concatenate([pvx, own_v], axis=0)),
                  "biasm": _bias_tables(rel_bias, j > 0), "onesr": np.ones((128, 64), f32)})
        maps.append(m)
    at = _run("aba", build_ab_attn, maps)
    if _debug is not None:
        _debug["haT"] = [ml[r]["haT"] for r in range(NCORES)]
        _debug["hbT"] = [at[r]["hbT"] for r in range(NCORES)]
    maps = []
    wo = ab_w_out[0]
    for r in range(NCORES):
        m = dict(mods1[r])
        m.update({"x": np.ascontiguousarray(xs[r]), "haT": ml[r]["haT"], "hbT": at[r]["hbT"],
                  "woa": np.ascontiguousarray(wo[0:512].reshape(4, 128, D).transpose(1, 0, 2)),
                  "wob": np.ascontiguousarray(wo[512:1024].reshape(8, 64, D).transpose(1, 0, 2))})
        maps.append(m)
    res = _run("abo", build_ab_out, maps)
    xs = [res[r]["y"] for r in range(NCORES)]
    dbg("l0s1", xs)
    xs = ffn_stage(xs, 0, 1)
    dbg("l0s2", xs)
    xs = ffn_stage(xs, 1, 0)
    dbg("l1s0", xs)
    maps = []
    for r, (b, j) in enumerate(cores):
        halo = xs[r - 1][TOK - 128:] if j > 0 else np.zeros((128, D), f32)
        maps.append(cd_inputs(xs[r], halo, j > 0, c[b], ada_w[1], ada_b[1], ln_g[1, 1], ln_b[1, 1], cd_w_in[0], cd_w_out[0],
                              cd_conv_w[0], cd_conv_b[0], cd_sgu_ln_g[0], cd_sgu_ln_b[0], cd_sgu_w[0], cd_sgu_b[0]))
    res = _run("cd", build_cd, maps)
    xs = [res[r]["y"] for r in range(NCORES)]
    dbg("l1s1", xs)
    xs = ffn_stage(xs, 1, 1)
    return np.stack(xs).reshape(2, SEQ, D).astype(np.float32)
```

```python
import numpy as np
import concourse.bass as bass
import concourse.mybir as mybir
from concourse.bass_utils import run_bass_kernel_spmd

F32 = mybir.dt.float32
BF16 = mybir.dt.bfloat16
I32 = mybir.dt.int32
ALU = mybir.AluOpType
AF = mybir.ActivationFunctionType

D = 1024
DFF = 2816
NCH = DFF // 128
SEQ = 8192
NCORES = 8
TOK = 2048
NT = TOK // 128
DEPTH = 2
ALPHA = float((2 * DEPTH) ** 0.25)
LN_EPS = 1e-5
FFN_RES_W = 0.5


class Sched:
    ENGS = ("tensor", "vector", "scalar", "gpsimd", "sync")

    def __init__(self):
        self.ops = {e: [] for e in self.ENGS}
        self.cnt = {}
        self.last_w = {}
        self.readers = {}
        self.known = {e: {} for e in self.ENGS}
        self.semkeys = []

    def _sem(self, key):
        if key not in self.cnt:
            self.cnt[key] = 0
            self.semkeys.append(key)

    def _deps(self, eng, r, w):
        deps = {}
        def add(d):
            if d is None:
                return
            key, val, src = d
            if src == "tensor" and eng == "tensor":
                return
            if src == "dma":
                val = self.cnt[key]
            if deps.get(key, 0) < val:
                deps[key] = val
        for res in r:
            add(self.last_w.get(res))
        for res in w:
            add(self.last_w.get(res))
            for d in self.readers.get(res, {}).values():
                add(d)
        waits = []
        for key, val in deps.items():
            if self.known[eng].get(key, 0) >= val:
                continue
            self.known[eng][key] = val
            waits.append((key, val))
        return waits

    def _commit(self, me, r, w):
        for res in w:
            self.last_w[res] = me
            self.readers[res] = {}
        for res in r:
            self.readers.setdefault(res, {})[me[0]] = me

    @staticmethod
    def _snap(fn):
        if fn is None or fn.__closure__ is None:
            return None
        out = []
        for name, c in zip(fn.__code__.co_freevars, fn.__closure__):
            try:
                v = c.cell_contents
            except ValueError:
                continue
            if isinstance(v, (int, float, str, slice, tuple)):
                out.append((name, v))
            elif not callable(v) and not isinstance(v, (dict, list)):
                out.append((name, id(v)))
        return out

    def op(self, eng, fn, r=(), w=()):
        self.snaps = getattr(self, "snaps", {})
        self.snaps[id(fn)] = (fn, self._snap(fn))
        key = "E:" + eng
        self._sem(key)
        waits = self._deps(eng, r, w)
        self.cnt[key] += 1
        me = (key, self.cnt[key], eng)
        self._commit(me, r, w)
        self.ops[eng].append((waits, fn, key, 1))

    def dma(self, queue, fn, slot, r=(), w=()):
        self.snaps = getattr(self, "snaps", {})
        self.snaps[id(fn)] = (fn, self._snap(fn))
        key = "D:" + slot
        self._sem(key)
        waits = self._deps(queue, r, w)
        self.cnt[key] += 16
        me = (key, self.cnt[key], "dma")
        self._commit(me, r, w)
        self.ops[queue].append((waits, fn, key, 16))

    def final_waits(self, eng, keys):
        waits = [(k, self.cnt[k]) for k in keys if self.cnt.get(k, 0) > 0]
        self.ops[eng].append((waits, None, None, 0))

    def emit(self, nc):
        from contextlib import ExitStack
        for fn, snap in getattr(self, "snaps", {}).values():
            now = self._snap(fn)
            if now != snap:
                raise RuntimeError(f"late-bound closure variable in {fn.__code__.co_filename}:{fn.__code__.co_firstlineno}: {snap} -> {now}")
        with ExitStack() as es:
            sems = {}
            for k in self.semkeys:
                sems[k] = es.enter_context(nc.semaphore(k.replace(":", "_")))
            block = es.enter_context(nc.Block())

            def runner(engname):
                def body(e):
                    for waits, fn, key, inc in self.ops[engname]:
                        for wk, wv in waits:
                            e.wait_ge(sems[wk], wv)
                        if fn is not None:
                            ins = fn(e)
                            ins.then_inc(sems[key], inc)
                return body

            block.tensor(runner("tensor"))
            block.vector(runner("vector"))
            block.scalar(runner("scalar"))
            block.gpsimd(runner("gpsimd"))
            block.sync(runner("sync"))


def emit_mod(S, nc, es, cT_d, adaw_d, adab_d, ps_banks, outs, ident_deps=()):
    raise NotImplementedError


def build_ffn(res_w=FFN_RES_W, ntiles=NT):
    nc = bass.Bass("TRN2", target_bir_lowering=False)
    ntok = ntiles * 128
    x_d = nc.dram_tensor("x", [ntok, D], F32, kind="ExternalInput").ap()
    cT_d = nc.dram_tensor("cT", [128, 8], F32, kind="ExternalInput").ap()
    adaw_d = nc.dram_tensor("adaw", [6, 128, 8, 512], F32, kind="ExternalInput").ap()
    adab_d = nc.dram_tensor("adab", [128, 3 * D], F32, kind="ExternalInput").ap()
    lng_d = nc.dram_tensor("lng", [128, D], F32, kind="ExternalInput").ap()
    lnb_d = nc.dram_tensor("lnb", [128, D], F32, kind="ExternalInput").ap()
    wg_d = nc.dram_tensor("wg", [NCH, 128, 8, 128], F32, kind="ExternalInput").ap()
    wu_d = nc.dram_tensor("wu", [NCH, 128, 8, 128], F32, kind="ExternalInput").ap()
    wd_d = nc.dram_tensor("wd", [NCH, 128, D], F32, kind="ExternalInput").ap()
    ident_d = nc.dram_tensor("ident", [128, 128], F32, kind="ExternalInput").ap()
    y_d = nc.dram_tensor("y", [ntok, D], F32, kind="ExternalOutput").ap()

    from contextlib import ExitStack
    S = Sched()
    with ExitStack() as es:
        def sb(name, shape, dt):
            return es.enter_context(nc.sbuf_tensor("s_" + name, shape, dt))
        xs = sb("xs", [128, ntiles, D], F32)
        emit_ffn_body(S, nc, es, sb, xs, ntiles, res_w, cT_d, adaw_d, adab_d, lng_d, lnb_d,
                      wg_d, wu_d, wd_d, ident_d, x_d, y_d)
        S.emit(nc)
    return nc


def emit_ffn_body(S, nc, es, sb, xs, ntiles, res_w, cT_d, adaw_d, adab_d, lng_d, lnb_d,
                  wg_d, wu_d, wd_d, ident_d, x_d, y_d):
    GT = 8
    ngroups = ntiles // GT
    HCH = NCH // 2
    hT = sb("hT", [128, 8, GT * 128], BF16)
    AT = sb("AT", [128, HCH, GT * 128], BF16)
    wdS2 = [sb(f"wdS{i}", [128, HCH, D], BF16) for i in range(2)]
    NRING = 3
    wgS = [sb(f"wgS{i}", [128, 8, 128], BF16) for i in range(NRING)]
    wuS = [sb(f"wuS{i}", [128, 8, 128], BF16) for i in range(NRING)]
    sc1 = sb("sc1", [128, D], F32)
    sh = sb("sh", [128, D], F32)
    gv = sb("gv", [128, D], F32)
    lng = sb("lng", [128, D], F32)
    lnb = sb("lnb", [128, D], F32)
    adab = sb("adab", [128, 512], F32)
    adaw = [AT[:, 4 * i:4 * i + 4, :].rearrange("p a b -> p (a b)").rearrange("p (k c) -> p k c", k=8) for i in range(2)]
    cT = sb("cTs", [128, 8], F32)
    csil = sb("csil", [128, 8], F32)
    csb = sb("csb", [128, 8, 128], BF16)
    ones_bf = sb("ones_bf", [128, 128], BF16)
    identf = sb("identf", [128, 128], F32)
    ident = sb("ident", [128, 128], BF16)
    tmp = [sb(f"tmp{i}", [128, D], F32) for i in range(2)]
    hb = [sb(f"hb{i}", [128, D], BF16) for i in range(2)]
    sg = [sb(f"sg{i}", [128, 512], F32) for i in range(2)]
    st6 = sb("st6", [128, 2, 6], F32)
    mv = sb("mv", [128, 2], F32)
    rstd = sb("rstd", [128, 1], F32)
    ps = [es.enter_context(nc.psum_tensor(f"ps{i}", [128, 512], F32)) for i in range(8)]

    S.dma("sync", lambda e: e.dma_start(out=identf[:], in_=ident_d[:, :]), "c0", w=["identf"])
    S.dma("sync", lambda e: e.dma_start(out=cT[:], in_=cT_d[:, :]), "c0", w=["cT"])
    S.dma("sync", lambda e: e.dma_start(out=lng[:], in_=lng_d[:, :]), "c1", w=["lng"])
    S.dma("sync", lambda e: e.dma_start(out=lnb[:], in_=lnb_d[:, :]), "c1", w=["lnb"])
    for q in range(4):
        t0, t1 = q * ntiles // 4, (q + 1) * ntiles // 4
        S.dma("sync", lambda e, t0=t0, t1=t1: e.dma_start(
            out=xs[:, t0:t1, :], in_=x_d[t0 * 128:t1 * 128, :].rearrange("(t p) d -> p t d", p=128)),
            f"x{q}", w=[("x", t) for t in range(t0, t1)])
    S.op("vector", lambda e: e.tensor_copy(ident[:], identf[:]), r=["identf"], w=["ident"])
    S.op("vector", lambda e: e.memset(ones_bf[:], 1.0), w=["ones_bf"])
    S.op("scalar", lambda e: e.activation(csil[:], cT[:], AF.Silu), r=["cT"], w=["csil"])
    for k in range(8):
        S.op("vector", lambda e, k=k: e.tensor_scalar(csb[:, k, :], ones_bf[:], csil[:, k:k + 1], None, ALU.mult),
             r=["csil", "ones_bf"], w=[("csb", k)])
    dsts = [sh, sh, sc1, sc1, gv, gv]
    for j in range(6):
        a = adaw[j % 2]
        S.dma("gpsimd", lambda e, a=a, j=j: e.dma_start(out=a, in_=adaw_d[j]), f"adaw{j % 2}", w=[("adaw", j % 2)])
        S.dma("sync", lambda e, j=j: e.dma_start(out=adab[:], in_=adab_d[:, j * 512:(j + 1) * 512]), "adab", w=["adab"])
        pb = ps[j % 2]
        for k in range(8):
            S.op("tensor", lambda e, a=a, k=k, pb=pb: e.matmul(pb[:], csb[:, k, :], a[:, k, :], start=(k == 0), stop=(k == 7)),
                 r=[("csb", k), ("adaw", j % 2)], w=[("ps", j % 2)])
        dst = dsts[j][:, (j % 2) * 512:(j % 2 + 1) * 512]
        dkey = ("modt", j)
        if j < 2:
            S.op("vector", lambda e, dst=dst, pb=pb: e.tensor_tensor(dst, pb[:], adab[:], ALU.add),
                 r=[("ps", j % 2), "adab"], w=[dkey])
        elif j < 4:
            S.op("vector", lambda e, dst=dst, pb=pb: e.scalar_tensor_tensor(dst, pb[:], 1.0, adab[:], ALU.add, ALU.add),
                 r=[("ps", j % 2), "adab"], w=[dkey])
        else:
            S.op("vector", lambda e, dst=dst, pb=pb: e.scalar_tensor_tensor(dst, pb[:], 1.0, adab[:], ALU.add, ALU.add),
                 r=[("ps", j % 2), "adab"], w=[dkey])
            S.op("vector", lambda e, dst=dst: e.tensor_scalar(dst, dst, float(res_w), None, ALU.mult),
                 r=[dkey], w=[dkey])
    MODR = [("modt", j) for j in range(6)]

    chunk_order = []
    for g in range(ngroups):
        for fh in range(2):
            for fc in range(HCH):
                chunk_order.append(fh * HCH + fc)
    nload = [0]

    def load_w(i):
        if i >= len(chunk_order):
            return
        c = chunk_order[i]
        e_ = i % NRING
        S.dma("gpsimd", lambda e, c=c, e_=e_: e.dma_start(out=wgS[e_][:], in_=wg_d[c]), f"w{e_}", w=[("wg", e_)])
        S.dma("gpsimd", lambda e, c=c, e_=e_: e.dma_start(out=wuS[e_][:], in_=wu_d[c]), f"w{e_}", w=[("wu", e_)])

    for i in range(NRING):
        load_w(i)

    mvg = sb("mvg", [128, GT, 2], F32)
    st6g = sb("st6g", [128, GT, 2, 6], F32)
    rstdg = sb("rstdg", [128, GT], F32)

    def emit_step1(g):
        for tt in range(GT):
            t = g * GT + tt
            tm, hbt = tmp[tt % 2], hb[tt % 2]
            pT = ps[2 + (tt % 2)]
            pTb = pT[:].bitcast(BF16)
            S.op("vector", lambda e, tm=tm, t=t: e.tensor_tensor(tm[:], xs[:, t, :], sc1[:], ALU.mult),
                 r=[("x", t)] + MODR, w=[("tmp", tt % 2)])
            S.op("gpsimd", lambda e, tm=tm, hbt=hbt: e.tensor_tensor(hbt[:], tm[:], sh[:], ALU.add),
                 r=[("tmp", tt % 2)] + MODR, w=[("hb", tt % 2)])
            for k in range(8):
                S.op("tensor", lambda e, k=k, hbt=hbt, pTb=pTb: e.transpose(pTb[:, k * 128:(k + 1) * 128], hbt[:, k * 128:(k + 1) * 128], ident[:]),
                     r=[("hb", tt % 2), "ident"], w=[("ps", 2 + (tt % 2))])
            S.op("scalar", lambda e, tt=tt, pTb=pTb: e.activation(
                hT[:, :, tt * 128:(tt + 1) * 128], pTb.rearrange("p (k t) -> p k t", k=8), AF.Copy),
                r=[("ps", 2 + (tt % 2))], w=[("hT", tt)])

    def emit_ln_tail(g):
        MVG = [("mvg", tt) for tt in range(GT)]
        S.op("vector", lambda e: e.tensor_scalar(rstdg[:], mvg[:, :, 1], LN_EPS, None, ALU.add), r=MVG, w=["rstdg"])
        S.op("scalar", lambda e: e.activation(rstdg[:], rstdg[:], AF.Sqrt), r=["rstdg"], w=["rstdg"])
        S.op("vector", lambda e: e.reciprocal(rstdg[:], rstdg[:]), r=["rstdg"], w=["rstdg"])
        for tt in range(GT):
            t = g * GT + tt
            xap = xs[:, t, :]
            S.op("vector", lambda e, xap=xap, tt=tt: e.tensor_scalar(xap, xap, mvg[:, tt, 0:1], rstdg[:, tt:tt + 1], ALU.subtract, ALU.mult),
                 r=[("mvg", tt), "rstdg", ("x", t)], w=[("x", t)])
            S.op("gpsimd", lambda e, xap=xap: e.tensor_tensor(xap, xap, lng[:], ALU.mult), r=[("x", t), "lng"], w=[("x", t)])
            S.op("gpsimd", lambda e, xap=xap: e.tensor_tensor(xap, xap, lnb[:], ALU.add), r=[("x", t), "lnb"], w=[("x", t)])
            S.dma("sync", lambda e, t=t: e.dma_start(out=y_d[t * 128:(t + 1) * 128, :], in_=xs[:, t, :]),
                  f"y{t % 4}", r=[("x", t)])

    ci = 0
    HTR = [("hT", tt) for tt in range(GT)]
    emit_step1(0)
    for g in range(ngroups):
        for fh in range(2):
            wdS = wdS2[fh]
            for fc in range(HCH):
                c = fh * HCH + fc
                S.dma("gpsimd", lambda e, c=c, fc=fc, wdS=wdS: e.dma_start(out=wdS[:, fc, :], in_=wd_d[c]), f"wd{fh}", w=[("wd", fh)])
            for fc in range(HCH):
                e_ = ci % NRING
                for hf in range(2):
                    slot = (fc * 2 + hf) % 2
                    pG, pU = ps[4 + slot * 2], ps[5 + slot * 2]
                    for k in range(8):
                        S.op("tensor", lambda e, k=k, e_=e_, hf=hf, pG=pG: e.matmul(
                            pG[:], wgS[e_][:, k, :], hT[:, k, hf * 512:(hf + 1) * 512], start=(k == 0), stop=(k == 7)),
                            r=[("wg", e_)] + HTR[hf * 4:(hf + 1) * 4], w=[("ps", 4 + slot * 2)])
                    for k in range(8):
                        S.op("tensor", lambda e, k=k, e_=e_, hf=hf, pU=pU: e.matmul(
                            pU[:], wuS[e_][:, k, :], hT[:, k, hf * 512:(hf + 1) * 512], start=(k == 0), stop=(k == 7)),
                            r=[("wu", e_)] + HTR[hf * 4:(hf + 1) * 4], w=[("ps", 5 + slot * 2)])
                    sgt = sg[slot]
                    S.op("scalar", lambda e, sgt=sgt, pG=pG: e.activation(sgt[:], pG[:], AF.Silu),
                         r=[("ps", 4 + slot * 2)], w=[("sg", slot)])
                    S.op("vector", lambda e, sgt=sgt, pU=pU, fc=fc, hf=hf: e.tensor_tensor(
                        AT[:, fc, hf * 512:(hf + 1) * 512], sgt[:], pU[:], ALU.mult),
                        r=[("sg", slot), ("ps", 5 + slot * 2)], w=[("AT", fc, hf)])
                load_w(ci + NRING)
                ci += 1
            for tt in range(GT):
                t = g * GT + tt
                hf = tt // 4
                pa = (tt % 2) * 2
                for dh in range(2):
                    pY = ps[pa + dh]
                    for fc in range(HCH):
                        S.op("tensor", lambda e, fc=fc, tt=tt, dh=dh, pY=pY, wdS=wdS: e.matmul(
                            pY[:], AT[:, fc, tt * 128:(tt + 1) * 128], wdS[:, fc, dh * 512:(dh + 1) * 512],
                            start=(fc == 0), stop=(fc == HCH - 1)),
                            r=[("AT", fc, hf), ("wd", fh)], w=[("ps", pa + dh)])
                tm = tmp[tt % 2]
                for dh in range(2):
                    S.op("vector", lambda e, tm=tm, dh=dh, pa=pa: e.tensor_tensor(
                        tm[:, dh * 512:(dh + 1) * 512], ps[pa + dh][:], gv[:, dh * 512:(dh + 1) * 512], ALU.mult),
                        r=[("ps", pa + dh)] + MODR, w=[("tmp", tt % 2, dh)])
                TM = [("tmp", tt % 2, 0), ("tmp", tt % 2, 1), ("tmp", tt % 2)]
                if fh == 0:
                    S.op("vector", lambda e, tm=tm, t=t: e.scalar_tensor_tensor(
                        xs[:, t, :], xs[:, t, :], ALPHA, tm[:], ALU.mult, ALU.add),
                        r=TM + [("x", t)], w=[("x", t), ("tmp", tt % 2)])
                else:
                    S.op("vector", lambda e, tm=tm, t=t: e.tensor_tensor(xs[:, t, :], xs[:, t, :], tm[:], ALU.add),
                         r=TM + [("x", t)], w=[("x", t), ("tmp", tt % 2)])
                    for hcol in range(2):
                        S.op("vector", lambda e, hcol=hcol, t=t, tt=tt: e.bn_stats(st6g[:, tt, hcol, :], xs[:, t, hcol * 512:(hcol + 1) * 512]),
                             r=[("x", t)], w=[("st6g", tt, hcol)])
                    S.op("vector", lambda e, tt=tt: e.bn_aggr(mvg[:, tt, :], st6g[:, tt, :, :]),
                         r=[("st6g", tt, 0), ("st6g", tt, 1)], w=[("mvg", tt)])
        if g + 1 < ngroups:
            emit_step1(g + 1)
        emit_ln_tail(g)
    S.final_waits("sync", [f"D:y{i}" for i in range(4)])


def emit_ln(S, xap, xkey, st6, mv, rstd, lng, lnb, eng="vector"):
    for hcol in range(2):
        S.op("vector", lambda e, hcol=hcol: e.bn_stats(st6[:, hcol, :], xap[:, hcol * 512:(hcol + 1) * 512]),
             r=[xkey], w=[("st6", hcol)])
    S.op("vector", lambda e: e.bn_aggr(mv[:], st6[:]), r=[("st6", 0), ("st6", 1)], w=["mv"])
    S.op("vector", lambda e: e.tensor_scalar(rstd[:], mv[:, 1:2], LN_EPS, None, ALU.add), r=["mv"], w=["rstd"])
    S.op("scalar", lambda e: e.activation(rstd[:], rstd[:], AF.Sqrt), r=["rstd"], w=["rstd"])
    S.op("vector", lambda e: e.reciprocal(rstd[:], rstd[:]), r=["rstd"], w=["rstd"])
    S.op("vector", lambda e: e.tensor_scalar(xap, xap, mv[:, 0:1], rstd[:, 0:1], ALU.subtract, ALU.mult),
         r=["mv", "rstd", xkey], w=[xkey])
    S.op("vector", lambda e: e.tensor_tensor(xap, xap, lng[:], ALU.mult), r=[xkey, "lng"], w=[xkey])
    S.op("vector", lambda e: e.tensor_tensor(xap, xap, lnb[:], ALU.add), r=[xkey, "lnb"], w=[xkey])


def lay_w_in(w, mc=128):
    F = w.shape[1]
    return np.ascontiguousarray(w.reshape(8, 128, F // mc, mc).transpose(2, 1, 0, 3))


def lay_adaw(w):
    return np.ascontiguousarray(w.reshape(8, 128, 6, 512).transpose(2, 1, 0, 3))


def ffn_inputs(x_sh, c_row, ada_w_l, ada_b_l, s, lng, lnb, wg, wu, wd):
    sl = slice(3 * s * D, 3 * (s + 1) * D)
    return {
        "x": np.ascontiguousarray(x_sh),
        "cT": np.ascontiguousarray(c_row.reshape(8, 128).T),
        "adaw": lay_adaw(ada_w_l[:, sl]),
        "adab": np.ascontiguousarray(np.broadcast_to(ada_b_l[sl][None, :], (128, 3 * D))),
        "lng": np.ascontiguousarray(np.broadcast_to(lng[None, :], (128, D))),
        "lnb": np.ascontiguousarray(np.broadcast_to(lnb[None, :], (128, D))),
        "wg": lay_w_in(wg), "wu": lay_w_in(wu),
        "wd": np.ascontiguousarray(wd.reshape(NCH, 128, D)),
        "ident": np.eye(128, dtype=np.float32),
    }


def emit_consts(S, sb, ident_d):
    identf = sb("identf", [128, 128], F32)
    ident = sb("ident", [128, 128], BF16)
    ones_bf = sb("ones_bf", [128, 128], BF16)
    S.dma("sync", lambda e: e.dma_start(out=identf[:], in_=ident_d[:, :]), "identf", w=["identf"])
    S.op("vector", lambda e: e.tensor_copy(ident[:], identf[:]), r=["identf"], w=["ident"])
    S.op("vector", lambda e: e.memset(ones_bf[:], 1.0), w=["ones_bf"])
    return ident, ones_bf


def emit_mod3(S, sb, ps, ones_bf, cT_d, adaw_d, adab_d, res_w, stage=None):
    sc1 = sb("sc1", [128, D], F32)
    sh = sb("sh", [128, D], F32)
    gv = sb("gv", [128, D], F32)
    adab = sb("adabS", [128, 512], F32)
    if stage is None:
        adaw_t = [sb(f"adawS{i}", [128, 8, 512], BF16) for i in range(2)]
        adaw = [t[:] for t in adaw_t]
        akeys = [("adaw", 0), ("adaw", 1)]
    else:
        adaw = [st[0] for st in stage]
        akeys = [st[1] for st in stage]
    cT = sb("cTs", [128, 8], F32)
    csil = sb("csil", [128, 8], F32)
    csb = sb("csb", [128, 8, 128], BF16)
    S.dma("sync", lambda e: e.dma_start(out=cT[:], in_=cT_d[:, :]), "cT", w=["cT"])
    S.op("scalar", lambda e: e.activation(csil[:], cT[:], AF.Silu), r=["cT"], w=["csil"])
    for k in range(8):
        S.op("vector", lambda e, k=k: e.tensor_scalar(csb[:, k, :], ones_bf[:], csil[:, k:k + 1], None, ALU.mult),
             r=["csil", "ones_bf"], w=[("csb", k)])
    dsts = [sh, sh, sc1, sc1, gv, gv]
    for j in range(6):
        a = adaw[j % 2]
        S.dma("gpsimd", lambda e, a=a, j=j: e.dma_start(out=a, in_=adaw_d[j]), f"adaw{j % 2}", w=[akeys[j % 2]])
        S.dma("sync", lambda e, j=j: e.dma_start(out=adab[:], in_=adab_d[:, j * 512:(j + 1) * 512]), "adab", w=["adab"])
        pb = ps[j % 2]
        for k in range(8):
            S.op("tensor", lambda e, a=a, k=k, pb=pb: e.matmul(pb[:], csb[:, k, :], a[:, k, :], start=(k == 0), stop=(k == 7)),
                 r=[("csb", k), akeys[j % 2]], w=[("ps", j % 2)])
        dst = dsts[j][:, (j % 2) * 512:(j % 2 + 1) * 512]
        dkey = ("modt", j)
        if j < 2:
            S.op("vector", lambda e, dst=dst, pb=pb: e.tensor_tensor(dst, pb[:], adab[:], ALU.add),
                 r=[("ps", j % 2), "adab"], w=[dkey])
        else:
            S.op("vector", lambda e, dst=dst, pb=pb: e.scalar_tensor_tensor(dst, pb[:], 1.0, adab[:], ALU.add, ALU.add),
                 r=[("ps", j % 2), "adab"], w=[dkey])
            if j >= 4 and res_w != 1.0:
                S.op("vector", lambda e, dst=dst: e.tensor_scalar(dst, dst, float(res_w), None, ALU.mult),
                     r=[dkey], w=[dkey])
    return sh, sc1, gv, [("modt", j) for j in range(6)]


def emit_h_transpose(S, xap, xkey, tmp, hb, slot, sc1, sh, MODR, ident, pT, pskey, dst_fn, dkey):
    tm, hbt = tmp[slot], hb[slot]
    pTb = pT[:].bitcast(BF16)
    S.op("vector", lambda e: e.tensor_tensor(tm[:], xap, sc1[:], ALU.mult), r=[xkey] + MODR, w=[("tmp", slot)])
    S.op("gpsimd", lambda e: e.tensor_tensor(hbt[:], tm[:], sh[:], ALU.add), r=[("tmp", slot)] + MODR, w=[("hb", slot)])
    for k in range(8):
        S.op("tensor", lambda e, k=k: e.transpose(pTb[:, k * 128:(k + 1) * 128], hbt[:, k * 128:(k + 1) * 128], ident[:]),
             r=[("hb", slot), "ident"], w=[pskey])
    S.op("scalar", lambda e: e.activation(dst_fn(), pTb.rearrange("p (k t) -> p k t", k=8), AF.Copy),
         r=[pskey], w=[dkey])


def emit_gelu(S, z, zkeys, out, outkeys, s1, s1key, n):
    S.op("scalar", lambda e: e.activation(s1[:, :n], z, AF.Square), r=zkeys, w=[s1key])
    S.op("vector", lambda e: e.tensor_scalar(s1[:, :n], s1[:, :n], 0.044715, 1.0, ALU.mult, ALU.add), r=[s1key], w=[s1key])
    S.op("vector", lambda e: e.tensor_tensor(s1[:, :n], s1[:, :n], z, ALU.mult), r=[s1key] + zkeys, w=[s1key])
    S.op("scalar", lambda e: e.activation(s1[:, :n], s1[:, :n], AF.Sigmoid, scale=1.5957691216057308), r=[s1key], w=[s1key])
    S.op("vector", lambda e: e.tensor_tensor(out, s1[:, :n], z, ALU.mult), r=[s1key] + zkeys, w=outkeys)


def emit_out_proj_ln(S, sb, ps, lhs_fn, lhs_keys_fn, woS, wokey, gv, MODR, lng, lnb, x_d, y_d, ntiles, tmp, xin, st6, mv, rstd):
    for t in range(ntiles):
        xt = xin[t % 2]
        S.dma("sync", lambda e, t=t, xt=xt: e.dma_start(out=xt[:], in_=x_d[t * 128:(t + 1) * 128, :]), f"xin{t % 2}", w=[("xin", t % 2)])
        pa = (t % 2) * 2
        for dh in range(2):
            for k in range(8):
                S.op("tensor", lambda e, t=t, dh=dh, k=k, pa=pa: e.matmul(
                    ps[pa + dh][:], lhs_fn(k, t), woS[:, k, dh * 512:(dh + 1) * 512], start=(k == 0), stop=(k == 7)),
                    r=lhs_keys_fn(k, t) + [wokey], w=[("ps", pa + dh)])
        tm = tmp[t % 2]
        for dh in range(2):
            S.op("vector", lambda e, tm=tm, dh=dh, pa=pa: e.tensor_tensor(
                tm[:, dh * 512:(dh + 1) * 512], ps[pa + dh][:], gv[:, dh * 512:(dh + 1) * 512], ALU.mult),
                r=[("ps", pa + dh)] + MODR, w=[("tmp", t % 2)])
        S.op("vector", lambda e, tm=tm, xt=xt: e.scalar_tensor_tensor(xt[:], xt[:], ALPHA, tm[:], ALU.mult, ALU.add),
             r=[("tmp", t % 2), ("xin", t % 2)], w=[("xin", t % 2)])
        emit_ln(S, xt[:], ("xin", t % 2), st6, mv, rstd, lng, lnb)
        S.dma("sync", lambda e, t=t, xt=xt: e.dma_start(out=y_d[t * 128:(t + 1) * 128, :], in_=xt[:]), f"yo{t % 2}", r=[("xin", t % 2)])
    S.final_waits("sync", ["D:yo0", "D:yo1"])


def build_cd():
    nc = bass.Bass("TRN2", target_bir_lowering=False)
    NTH = NT + 1
    x_d = nc.dram_tensor("x", [TOK, D], F32, kind="ExternalInput").ap()
    xh_d = nc.dram_tensor("xh", [128, D], F32, kind="ExternalInput").ap()
    flag_d = nc.dram_tensor("flag", [128, 1], F32, kind="ExternalInput").ap()
    cT_d = nc.dram_tensor("cT", [128, 8], F32, kind="ExternalInput").ap()
    adaw_d = nc.dram_tensor("adaw", [6, 128, 8, 512], F32, kind="ExternalInput").ap()
    adab_d = nc.dram_tensor("adab", [128, 3 * D], F32, kind="ExternalInput").ap()
    lng_d = nc.dram_tensor("lng", [128, D], F32, kind="ExternalInput").ap()
    lnb_d = nc.dram_tensor("lnb", [128, D], F32, kind="ExternalInput").ap()
    win_d = nc.dram_tensor("win", [16, 128, 8, 128], F32, kind="ExternalInput").ap()
    wv_d = nc.dram_tensor("wv", [128, 8, 512], F32, kind="ExternalInput").ap()
    wo_d = nc.dram_tensor("wo", [128, 8, D], F32, kind="ExternalInput").ap()
    cw_d = nc.dram_tensor("cw", [128, 4, 4], F32, kind="ExternalInput").ap()
    sg_d = nc.dram_tensor("sgg", [128, 512], F32, kind="ExternalInput").ap()
    sbb_d = nc.dram_tensor("sgb", [128, 512], F32, kind="ExternalInput").ap()
    wm_d = nc.dram_tensor("wm", [128, 4, 128], F32, kind="ExternalInput").ap()
    msk_d = nc.dram_tensor("msk", [128, 128], F32, kind="ExternalInput").ap()
    sgub_d = nc.dram_tensor("sgub", [128, 4, 512], F32, kind="ExternalInput").ap()
    ident_d = nc.dram_tensor("ident", [128, 128], F32, kind="ExternalInput").ap()
    y_d = nc.dram_tensor("y", [TOK, D], F32, kind="ExternalOutput").ap()

    from contextlib import ExitStack
    S = Sched()
    with ExitStack() as es:
        def sb(name, shape, dt):
            return es.enter_context(nc.sbuf_tensor("s_" + name, shape, dt))
        ps = [es.enter_context(nc.psum_tensor(f"ps{i}", [128, 512], F32)) for i in range(8)]
        ident, ones_bf = emit_consts(S, sb, ident_d)
        acc = sb("acc", [128, TOK], F32)
        gu = sb("gu", [128, TOK], F32)
        stage = [(acc[:].bitcast(BF16).rearrange("p (k c) -> p k c", k=8), "acc"),
                 (gu[:].bitcast(BF16).rearrange("p (k c) -> p k c", k=8), ("gu", 0))]
        sh, sc1, gv, MODR = emit_mod3(S, sb, ps, ones_bf, cT_d, adaw_d, adab_d, 1.0, stage=stage)
        lng = sb("lngS", [128, D], F32)
        lnb = sb("lnbS", [128, D], F32)
        S.dma("sync", lambda e: e.dma_start(out=lng[:], in_=lng_d[:, :]), "lng", w=["lng"])
        S.dma("sync", lambda e: e.dma_start(out=lnb[:], in_=lnb_d[:, :]), "lnb", w=["lnb"])
        hT = sb("hT", [128, 8, NTH * 128], BF16)
        ycT = sb("ycT", [128, 4, TOK], BF16)
        ydT = sb("ydT", [128, 4, TOK], BF16)
        vn = sb("vn", [128, NT, 512], BF16)
        woS = sb("woS", [128, 8, D], BF16)
        wvS = sb("wvS", [128, 8, 512], BF16)
        NR = 3
        wr = [sb(f"wr{i}", [128, 8, 128], BF16) for i in range(NR)]
        pT_ = sb("pT", [128, 128 + TOK], F32)
        s1 = [sb(f"s1_{i}", [128, 512], F32) for i in range(2)]
        gcs = [sb(f"gcs{i}", [128, 128], F32) for i in range(1)]
        vg = [sb(f"vg{i}", [128, 512], F32) for i in range(2)]
        tmp = [sb(f"tmp{i}", [128, D], F32) for i in range(2)]
        hb = [sb(f"hb{i}", [128, D], BF16) for i in range(2)]
        xin = [sb(f"xin{i}", [128, D], F32) for i in range(2)]
        cw = sb("cwS", [128, 4, 4], F32)
        flag = sb("flagS", [128, 1], F32)
        sgg = sb("sggS", [128, 512], F32)
        sgb = sb("sgbS", [128, 512], F32)
        wmf = sb("wmf", [128, 4, 128], F32)
        mskS = sb("mskS", [128, 128], F32)
        wmT = sb("wmT", [128, 4, 128], BF16)
        sgub = sb("sgubS", [128, 4, 512], F32)
        st6 = sb("st6", [128, 4, 6], F32)
        mv = sb("mv", [128, 4, 2], F32)
        rstd = sb("rstd", [128, 4], F32)

        for (t_, d_, nm) in [(cw, cw_d, "cw"), (flag, flag_d, "flag"), (sgg, sg_d, "sgg"), (sgb, sbb_d, "sgb"),
                             (wmf, wm_d, "wmf"), (mskS, msk_d, "msk"), (sgub, sgub_d, "sgub")]:
            S.dma("sync", lambda e, t_=t_, d_=d_: e.dma_start(out=t_[:], in_=d_), nm, w=[nm])
        S.dma("gpsimd", lambda e: e.dma_start(out=wvS[:], in_=wv_d), "wv", w=["wv"])
        S.dma("gpsimd", lambda e: e.dma_start(out=woS[:], in_=wo_d), "wo", w=["wo"])
        for g in range(4):
            S.op("vector", lambda e, g=g: e.tensor_tensor(wmT[:, g, :], wmf[:, g, :], mskS[:], ALU.mult),
                 r=["wmf", "msk"], w=[("wmT", g)])

        for i in range(NTH):
            xt = xin[i % 2]
            src = xh_d if i == 0 else x_d[(i - 1) * 128:i * 128, :]
            S.dma("sync", lambda e, xt=xt, src=src: e.dma_start(out=xt[:], in_=src), f"xin{i % 2}", w=[("xin", i % 2)])
            emit_h_transpose(S, xt[:], ("xin", i % 2), tmp, hb, i % 2, sc1, sh, MODR, ident, ps[2 + i % 2], ("ps", 2 + i % 2),
                             (lambda i=i: hT[:, :, i * 128:(i + 1) * 128]), ("hT", i))
        HT_ALL = [("hT", i) for i in range(NTH)]

        def piece_keys(pc):
            return [("hT", 1 + pc * 4 + a) for a in range(4)]

        for t in range(NT):
            pv = ps[4 + t % 2]
            for k in range(8):
                S.op("tensor", lambda e, t=t, k=k, pv=pv: e.matmul(pv[:], hT[:, k, (t + 1) * 128:(t + 2) * 128], wvS[:, k, :],
                                                              start=(k == 0), stop=(k == 7)),
                     r=[("hT", t + 1), "wv"], w=[("ps", 4 + t % 2)])
            vgt = vg[t % 2]
            emit_gelu(S, pv[:], [("ps", 4 + t % 2)], vgt[:], [("vg", t % 2)], s1[t % 2], ("s1", t % 2), 512)
            for g in range(4):
                S.op("vector", lambda e, g=g, vgt=vgt: e.bn_stats(st6[:, g, :], vgt[:, g * 128:(g + 1) * 128]),
                     r=[("vg", t % 2)], w=[("st6", g)])
                S.op("vector", lambda e, g=g: e.bn_aggr(mv[:, g, :], st6[:, g, :]), r=[("st6", g)], w=[("mv", g)])
            MV = [("mv", g) for g in range(4)]
            S.op("vector", lambda e: e.tensor_scalar(rstd[:], mv[:, :, 1], LN_EPS, None, ALU.add), r=MV, w=["rstd"])
            S.op("scalar", lambda e: e.activation(rstd[:], rstd[:], AF.Sqrt), r=["rstd"], w=["rstd"])
            S.op("vector", lambda e: e.reciprocal(rstd[:], rstd[:]), r=["rstd"], w=["rstd"])
            for g in range(4):
                S.op("vector", lambda e, g=g, vgt=vgt: e.tensor_scalar(vgt[:, g * 128:(g + 1) * 128], vgt[:, g * 128:(g + 1) * 128],
                                                                    mv[:, g, 0:1], rstd[:, g:g + 1], ALU.subtract, ALU.mult),
                     r=[("vg", t % 2), "rstd"] + MV, w=[("vg", t % 2)])
            S.op("vector", lambda e, vgt=vgt: e.tensor_tensor(vgt[:], vgt[:], sgg[:], ALU.mult), r=[("vg", t % 2), "sgg"], w=[("vg", t % 2)])
            S.op("vector", lambda e, vgt=vgt, t=t: e.tensor_tensor(vn[:, t, :], vgt[:], sgb[:], ALU.add),
                 r=[("vg", t % 2), "sgb"], w=[("vn", t)])

        order = []
        for q in range(4):
            order += [4 + q, 8 + q, q]
        for g in range(4):
            order.append(12 + g)
        state = {"i": 0}

        def load_next():
            i = state["i"]
            if i >= len(order):
                return
            c = order[i]
            S.dma("gpsimd", lambda e, c=c, i=i: e.dma_start(out=wr[i % NR][:], in_=win_d[c]), f"wr{i % NR}", w=[("wr", i % NR)])
            state["i"] += 1

        for _ in range(NR):
            load_next()
        use = {"i": 0}

        def proj(pbank, pkey, col0, ncol, hkeys):
            slot = use["i"] % NR
            for k in range(8):
                S.op("tensor", lambda e, k=k, slot=slot: e.matmul(pbank[:, :ncol], wr[slot][:, k, :], hT[:, k, col0:col0 + ncol],
                                                                 start=(k == 0), stop=(k == 7)),
                     r=[("wr", slot)] + hkeys, w=[pkey])

        def done_chunk():
            use["i"] += 1
            load_next()

        for q in range(4):
            pieces = [(0, 128, [("hT", 0)])] + [(128 + pc * 512, 512, piece_keys(pc)) for pc in range(4)]
            gc_tiles = []
            for pi, (c0, ncol, hk) in enumerate(pieces):
                pb = ps[pi % 2]
                proj(pb, ("ps", pi % 2), c0, ncol, hk)
                S.op("scalar", lambda e, pb=pb, c0=c0, ncol=ncol: e.activation(gu[:, c0 - 128:c0 - 128 + ncol] if c0 >= 128 else gcs[0][:, :ncol],
                                                                              pb[:, :ncol], AF.Copy),
                     r=[("ps", pi % 2)], w=[(("gu", pi - 1) if pi >= 1 else "gcs0")])
            done_chunk()
            for pi, (c0, ncol, hk) in enumerate(pieces):
                pb = ps[2 + pi % 2]
                proj(pb, ("ps", 2 + pi % 2), c0, ncol, hk)
                gsrc = (gu[:, c0 - 128:c0 - 128 + ncol] if c0 >= 128 else gcs[0][:, :ncol])
                S.op("vector", lambda e, pb=pb, c0=c0, ncol=ncol, gsrc=gsrc: e.tensor_tensor(pT_[:, c0:c0 + ncol], gsrc, pb[:, :ncol], ALU.mult),
                     r=[("ps", 2 + pi % 2), (("gu", pi - 1) if pi >= 1 else "gcs0")], w=[("pT", pi)])
            done_chunk()
            PT = [("pT", pi) for pi in range(5)]
            S.op("vector", lambda e: e.tensor_scalar(pT_[:, 0:128], pT_[:, 0:128], flag[:, 0:1], None, ALU.mult),
                 r=[("pT", 0), "flag"], w=[("pT", 0)])
            S.op("vector", lambda e, q=q: e.tensor_scalar(acc[:], pT_[:, 126:126 + TOK], cw[:, q, 0:1], cw[:, q, 3:4], ALU.mult, ALU.add),
                 r=PT + ["cw"], w=["acc"])
            S.op("vector", lambda e, q=q: e.scalar_tensor_tensor(acc[:], pT_[:, 127:127 + TOK], cw[:, q, 1:2], acc[:], ALU.mult, ALU.add),
                 r=PT + ["cw", "acc"], w=["acc"])
            S.op("vector", lambda e, q=q: e.scalar_tensor_tensor(acc[:], pT_[:, 128:128 + TOK], cw[:, q, 2:3], acc[:], ALU.mult, ALU.add),
                 r=PT + ["cw", "acc"], w=["acc"])
            for pc in range(4):
                pb = ps[pc % 2]
                proj(pb, ("ps", pc % 2), 128 + pc * 512, 512, piece_keys(pc))
                S.op("vector", lambda e, pb=pb, pc=pc, q=q: e.tensor_tensor(ycT[:, q, pc * 512:(pc + 1) * 512], acc[:, pc * 512:(pc + 1) * 512], pb[:], ALU.mult),
                     r=[("ps", pc % 2), "acc"], w=[("ycT", q, pc)])
            done_chunk()

        for g in range(4):
            for pc in range(4):
                pb = ps[pc % 2]
                proj(pb, ("ps", pc % 2), 128 + pc * 512, 512, piece_keys(pc))
                emit_gelu(S, pb[:], [("ps", pc % 2)], gu[:, pc * 512:(pc + 1) * 512], [("gu", pc)], s1[pc % 2], ("s1", pc % 2), 512)
            done_chunk()
            for pc in range(4):
                pm = ps[2 + pc % 2]
                for a in range(4):
                    t = pc * 4 + a
                    S.op("tensor", lambda e, a=a, t=t, g=g, pm=pm: e.matmul(pm[:, a * 128:(a + 1) * 128], vn[:, t, g * 128:(g + 1) * 128], wmT[:, g, :],
                                                                         start=True, stop=True),
                         r=[("vn", t), ("wmT", g)], w=[("ps", 2 + pc % 2)])
                s1t = s1[pc % 2]
                S.op("vector", lambda e, pm=pm, g=g, s1t=s1t: e.tensor_tensor(s1t[:], pm[:], sgub[:, g, :], ALU.add),
                     r=[("ps", 2 + pc % 2), "sgub"], w=[("s1", pc % 2)])
                S.op("vector", lambda e, g=g, pc=pc, s1t=s1t: e.tensor_tensor(ydT[:, g, pc * 512:(pc + 1) * 512], s1t[:], gu[:, pc * 512:(pc + 1) * 512], ALU.mult),
                     r=[("s1", pc % 2), ("gu", pc)], w=[("ydT", g, pc)])

        def lhs_fn(k, t):
            return (ycT[:, k, t * 128:(t + 1) * 128] if k < 4 else ydT[:, k - 4, t * 128:(t + 1) * 128])

        def lhs_keys(k, t):
            return [("ycT", k, t // 4)] if k < 4 else [("ydT", k - 4, t // 4)]
        emit_out_proj_ln(S, sb, ps, lhs_fn, lhs_keys, woS, "wo", gv, MODR, lng, lnb, x_d, y_d, NT, tmp, xin, st6[:, 0:2, :], mv[:, 0, :], rstd[:, 0:1])
        S.emit(nc)
    return nc


def lay_cols_rhs(w):
    return np.ascontiguousarray(w.reshape(8, 128, w.shape[1]).transpose(1, 0, 2))


def mod_inputs(c_row, ada_w_l, ada_b_l, s, lng, lnb):
    sl = slice(3 * s * D, 3 * (s + 1) * D)
    return {
        "cT": np.ascontiguousarray(c_row.reshape(8, 128).T),
        "adaw": lay_adaw(ada_w_l[:, sl]),
        "adab": np.ascontiguousarray(np.broadcast_to(ada_b_l[sl][None, :], (128, 3 * D))),
        "lng": np.ascontiguousarray(np.broadcast_to(lng[None, :], (128, D))),
        "lnb": np.ascontiguousarray(np.broadcast_to(lnb[None, :], (128, D))),
        "ident": np.eye(128, dtype=np.float32),
    }


def cd_inputs(x_sh, x_halo, has_prev, c_row, ada_w_l, ada_b_l, lng, lnb, w_in, w_out, conv_w, conv_b, sln_g, sln_b, sgu_w, sgu_b):
    m = mod_inputs(c_row, ada_w_l, ada_b_l, 1, lng, lnb)
    cw = np.zeros((128, 4, 4), np.float32)
    cw[:, :, 0:3] = conv_w.T.reshape(4, 128, 3).transpose(1, 0, 2)
    cw[:, :, 3] = conv_b.reshape(4, 128).T
    tri = (np.arange(128)[:, None] <= np.arange(128)[None, :]).astype(np.float32)
    m.update({
        "x": np.ascontiguousarray(x_sh), "xh": np.ascontiguousarray(x_halo),
        "flag": np.full((128, 1), 1.0 if has_prev else 0.0, np.float32),
        "win": lay_w_in(w_in[:, :2048]), "wv": lay_cols_rhs(w_in[:, 2048:2560]), "wo": lay_cols_rhs(w_out),
        "cw": cw,
        "sgg": np.ascontiguousarray(np.broadcast_to(sln_g.reshape(1, 512), (128, 512))),
        "sgb": np.ascontiguousarray(np.broadcast_to(sln_b.reshape(1, 512), (128, 512))),
        "wm": np.ascontiguousarray(sgu_w.transpose(2, 0, 1)),
        "msk": tri,
        "sgub": np.ascontiguousarray(np.broadcast_to(sgu_b[None, :, None, :], (128, 4, 4, 128)).reshape(128, 4, 512)),
    })
    return m


class Launch:
    def __init__(self):
        from contextlib import ExitStack
        self.nc = bass.Bass("TRN2", target_bir_lowering=False)
        self.S = Sched()
        self.es = ExitStack()
        self.ps = [self.es.enter_context(self.nc.psum_tensor(f"ps{i}", [128, 512], F32)) for i in range(8)]

    def sb(self, name, shape, dt):
        return self.es.enter_context(self.nc.sbuf_tensor("s_" + name, shape, dt))

    def din(self, name, shape, dt=F32):
        return self.nc.dram_tensor(name, list(shape), dt, kind="ExternalInput").ap()

    def dout(self, name, shape, dt=F32):
        return self.nc.dram_tensor(name, list(shape), dt, kind="ExternalOutput").ap()

    def load(self, name, shape, dt, src, queue="sync", key=None):
        t = self.sb(name, shape, dt)
        self.S.dma(queue, lambda e: e.dma_start(out=t[:], in_=src), name, w=[key or name])
        return t

    def finish(self):
        self.S.emit(self.nc)
        self.es.close()
        return self.nc


def launch_prologue(L, res_w=1.0, want_ln=False, stage=None):
    x_d = L.din("x", [TOK, D])
    cT_d = L.din("cT", [128, 8])
    adaw_d = L.din("adaw", [6, 128, 8, 512])
    adab_d = L.din("adab", [128, 3 * D])
    lng_d = L.din("lng", [128, D])
    lnb_d = L.din("lnb", [128, D])
    ident_d = L.din("ident", [128, 128])
    ident, ones_bf = emit_consts(L.S, L.sb, ident_d)
    sh, sc1, gv, MODR = emit_mod3(L.S, L.sb, L.ps, ones_bf, cT_d, adaw_d, adab_d, res_w, stage=stage)
    lng = lnb = None
    if want_ln:
        lng = L.load("lngS", [128, D], F32, lng_d[:, :], key="lng")
        lnb = L.load("lnbS", [128, D], F32, lnb_d[:, :], key="lnb")
    return dict(x_d=x_d, ident=ident, ones_bf=ones_bf, sh=sh, sc1=sc1, gv=gv, MODR=MODR, lng=lng, lnb=lnb)


def emit_hT_all(L, P, hT, ntiles=NT):
    S = L.S
    tmp = [L.sb(f"tmp{i}", [128, D], F32) for i in range(2)]
    hb = [L.sb(f"hb{i}", [128, D], BF16) for i in range(2)]
    xin = [L.sb(f"xin{i}", [128, D], F32) for i in range(2)]
    for i in range(ntiles):
        xt = xin[i % 2]
        S.dma("sync", lambda e, xt=xt, i=i: e.dma_start(out=xt[:], in_=P["x_d"][i * 128:(i + 1) * 128, :]), f"xin{i % 2}", w=[("xin", i % 2)])
        emit_h_transpose(S, xt[:], ("xin", i % 2), tmp, hb, i % 2, P["sc1"], P["sh"], P["MODR"], P["ident"], L.ps[2 + i % 2],
                         ("ps", 2 + i % 2), (lambda i=i: hT[:, :, i * 128:(i + 1) * 128]), ("hT", i))
    return tmp, hb, xin


def emit_featmajor_proj(L, w_d, nchunks, hT, dst, dkey, ps_ids=(0, 1), nring=2, name="wf", mc=128):
    S = L.S
    wr = [L.sb(f"{name}{i}", [128, 8, mc], BF16) for i in range(nring)]
    for c in range(min(nring, nchunks)):
        S.dma("gpsimd", lambda e, c=c: e.dma_start(out=wr[c % nring][:], in_=w_d[c]), f"{name}{c % nring}", w=[(name, c % nring)])
    for c in range(nchunks):
        slot = c % nring
        for pc in range(TOK // 512):
            pid = ps_ids[pc % len(ps_ids)]
            pb = L.ps[pid]
            for k in range(8):
                S.op("tensor", lambda e, k=k, slot=slot, pc=pc, pb=pb: e.matmul(pb[0:mc, :], wr[slot][:, k, :], hT[:, k, pc * 512:(pc + 1) * 512],
                                                                             start=(k == 0), stop=(k == 7)),
                     r=[(name, slot)] + [("hT", pc * 4 + a) for a in range(4)], w=[("ps", pid)])
            S.op("scalar", lambda e, c=c, pc=pc, pb=pb: e.activation(dst[:, c, pc * 512:(pc + 1) * 512], pb[0:mc, :], AF.Copy),
                 r=[("ps", pid)], w=[(dkey, c, pc)])
        if c + nring < nchunks:
            c2 = c + nring
            S.dma("gpsimd", lambda e, c2=c2: e.dma_start(out=wr[c2 % nring][:], in_=w_d[c2]), f"{name}{c2 % nring}", w=[(name, c2 % nring)])


def emit_mlstm_chunk_prep(L, t, hT, W, C, pools):
    S = L.S
    ps = L.ps
    hk = [("hT", t)]
    col = slice(t * 128, (t + 1) * 128)
    gsb, sp, e1, tsum, bs, thr, expF, mk_sb, Vp, tC = (pools[n] for n in ("gsb", "sp", "e1", "tsum", "bs", "thr", "expF", "mk_sb", "Vp", "tC"))
    for k in range(8):
        S.op("tensor", lambda e, k=k: e.matmul(ps[0][:, 0:8], hT[:, k, col], W["wgt"][:, k, :], start=(k == 0), stop=(k == 7)),
             r=hk + ["wgt"], w=[("ps", 0)])
    S.op("vector", lambda e: e.tensor_tensor(gsb[:], ps[0][:, 0:8], W["bgate"][:], ALU.add), r=[("ps", 0), "bgate"], w=["gsb"])
    S.op("scalar", lambda e: e.activation(e1[:], gsb[:, 4:8], AF.Exp, scale=-1.0), r=["gsb"], w=["e1"])
    S.op("scalar", lambda e: e.activation(sp[:], e1[:], AF.Ln, bias=1.0), r=["e1"], w=["sp"])
    S.op("tensor", lambda e: e.matmul(ps[0][:, 16:20], W["tri"][:], sp[:], start=True, stop=True), r=["tri", "sp"], w=[("ps", 0)])
    S.op("tensor", lambda e: e.matmul(ps[0][:, 24:28], W["onesf"][:], sp[:], start=True, stop=True), r=["onesf", "sp"], w=[("ps", 0)])
    S.op("vector", lambda e: e.tensor_tensor(tsum[:], gsb[:, 0:4], ps[0][:, 16:20], ALU.add), r=["gsb", ("ps", 0)], w=["tsum"])
    S.op("scalar", lambda e: e.activation(bs[:], tsum[:], AF.Exp, bias=float(-np.log(8.0))), r=["tsum"], w=["bs"])
    S.op("scalar", lambda e: e.activation(thr[:], ps[0][:, 16:20], AF.Exp), r=[("ps", 0)], w=["thr"])
    S.op("scalar", lambda e: e.activation(expF[:], ps[0][0:64, 24:28], AF.Exp, scale=-1.0), r=[("ps", 0)], w=["expF"])
    for k in range(8):
        S.op("tensor", lambda e, k=k: e.matmul(ps[1][:], hT[:, k, col], W["wmv"][:, k, :], start=(k == 0), stop=(k == 7)),
             r=hk + ["wmv"], w=[("ps", 1)])
    for h in range(4):
        S.op("vector", lambda e, h=h: e.tensor_scalar(Vp[:, h, 0:128], ps[1][:, h * 128:(h + 1) * 128], bs[:, h:h + 1], None, ALU.mult),
             r=[("ps", 1), "bs"], w=[("Vp", h)])
    S.op("vector", lambda e: e.tensor_copy(Vp[:, :, 128], bs[:]), r=["bs"] + [("Vp", h) for h in range(4)], w=[("Vp", h) for h in range(4)])
    for k in range(8):
        S.op("tensor", lambda e, k=k: e.matmul(ps[0][:, 256:512], hT[:, k, col], W["wmk"][:, k, :], start=(k == 0), stop=(k == 7)),
             r=hk + ["wmk"], w=[("ps", 0)])
    S.op("scalar", lambda e: e.activation(mk_sb[:], ps[0][:, 256:512], AF.Copy), r=[("ps", 0)], w=["mk_sb"])


def emit_mlstm_state_update(L, C, pools):
    S = L.S
    ps = L.ps
    mk_sb, Vp, expF, tC = pools["mk_sb"], pools["Vp"], pools["expF"], pools["tC"]
    for h in range(4):
        pb = ps[6 + h // 2]
        S.op("tensor", lambda e, h=h, pb=pb: e.matmul(pb[0:64, (h % 2) * 129:(h % 2) * 129 + 129], mk_sb[:, h * 64:(h + 1) * 64], Vp[:, h, :],
                                                     start=True, stop=True),
             r=["mk_sb", ("Vp", h)], w=[("ps", 6 + h // 2)])
    for h in range(4):
        pb = ps[6 + h // 2]
        S.op("vector", lambda e, h=h, pb=pb: e.tensor_tensor(tC[:, h, :], C[:, h, :], pb[0:64, (h % 2) * 129:(h % 2) * 129 + 129], ALU.add),
             r=[("ps", 6 + h // 2), ("C", h)], w=[("tC", h)])
        S.op("vector", lambda e, h=h: e.tensor_scalar(C[:, h, :], tC[:, h, :], expF[:, h:h + 1], None, ALU.mult),
             r=[("tC", h), "expF"], w=[("C", h)])


def mlstm_pools(L):
    p = {}
    p["gsb"] = L.sb("gsb", [128, 8], F32)
    p["sp"] = L.sb("sp", [128, 4], F32)
    p["e1"] = L.sb("e1", [128, 4], F32)
    p["tsum"] = L.sb("tsum", [128, 4], F32)
    p["bs"] = L.sb("bs", [128, 4], F32)
    p["thr"] = L.sb("thr", [128, 4], F32)
    p["expF"] = L.sb("expF", [64, 4], F32)
    p["mk_sb"] = L.sb("mk_sb", [128, 256], BF16)
    p["Vp"] = L.sb("Vp", [128, 4, 129], BF16)
    p["tC"] = L.sb("tC", [64, 4, 129], F32)
    return p


def mlstm_weights(L):
    W = {}
    wmk_d = L.din("wmk", [128, 8, 256]); wmv_d = L.din("wmv", [128, 8, 512]); wgt_d = L.din("wgt", [128, 8, 8])
    W["wmk"] = L.load("wmk", [128, 8, 256], BF16, wmk_d, "gpsimd")
    W["wmv"] = L.load("wmv", [128, 8, 512], BF16, wmv_d, "gpsimd")
    W["wgt"] = L.load("wgt", [128, 8, 8], BF16, wgt_d, "gpsimd")
    W["bgate"] = L.load("bgate", [128, 8], F32, L.din("bgate", [128, 8]))
    W["tri"] = L.load("tri", [128, 128], F32, L.din("tri", [128, 128]))
    W["onesf"] = L.load("onesf", [128, 128], F32, L.din("onesf", [128, 128]))
    return W


def build_ab_producer():
    L = Launch()
    S = L.S
    P = launch_prologue(L)
    wk_d = L.din("wk", [8, 128, 8, 64])
    wav_d = L.din("wav", [128, 8, 512])
    kT_d = L.dout("kT", [64, 8, TOK], BF16)
    vx_d = L.dout("vx", [TOK, 520], BF16)
    cseg_d = L.dout("cseg", [64, 4 * 129])
    sseg_d = L.dout("sseg", [64, 4])
    hT = L.sb("hT", [128, 8, TOK], BF16)
    emit_hT_all(L, P, hT)
    kTs = L.sb("kTs", [64, 8, TOK], BF16)
    emit_featmajor_proj(L, wk_d, 8, hT, kTs, "kTs", ps_ids=(4, 5), mc=64)
    S.dma("sync", lambda e: e.dma_start(out=kT_d, in_=kTs[:]), "kTout", r=[("kTs", c, pc) for c in range(8) for pc in range(4)])
    wav = L.load("wav", [128, 8, 512], BF16, wav_d, "gpsimd")
    W = mlstm_weights(L)
    pools = mlstm_pools(L)
    C = L.sb("Cst", [64, 4, 129], F32)
    ssum = L.sb("ssum", [64, 4], F32)
    S.op("vector", lambda e: e.memset(C[:], 0.0), w=[("C", h) for h in range(4)])
    S.op("vector", lambda e: e.memset(ssum[:], 0.0), w=["ssum"])
    vxs = [L.sb(f"vxs{i}", [128, 8, 65], BF16) for i in range(2)]
    for i in range(2):
        S.op("vector", lambda e, i=i: e.memset(vxs[i][:], 1.0), w=[("vxs", i)])
    lnexp = L.sb("lnexp", [64, 4], F32)
    for t in range(NT):
        col = slice(t * 128, (t + 1) * 128)
        for k in range(8):
            S.op("tensor", lambda e, k=k, col=col, t=t: e.matmul(L.ps[4 + t % 2][:], hT[:, k, col], wav[:, k, :], start=(k == 0), stop=(k == 7)),
                 r=[("hT", t), "wav"], w=[("ps", 4 + t % 2)])
        vt = vxs[t % 2]
        S.op("scalar", lambda e, vt=vt, t=t: e.activation(vt[:, :, 0:64], L.ps[4 + t % 2][:].rearrange("p (h c) -> p h c", h=8), AF.Copy),
             r=[("ps", 4 + t % 2)], w=[("vxs", t % 2)])
        S.dma("sync", lambda e, vt=vt, t=t: e.dma_start(out=vx_d[t * 128:(t + 1) * 128, :], in_=vt[:].rearrange("p h c -> p (h c)")),
              f"vxo{t % 2}", r=[("vxs", t % 2)])
        emit_mlstm_chunk_prep(L, t, hT, W, C, pools)
        emit_mlstm_state_update(L, C, pools)
        S.op("scalar", lambda e: e.activation(lnexp[:], pools["expF"][:], AF.Ln), r=["expF"], w=["lnexp"])
        S.op("vector", lambda e: e.tensor_tensor(ssum[:], ssum[:], lnexp[:], ALU.subtract), r=["lnexp", "ssum"], w=["ssum"])
    S.dma("sync", lambda e: e.dma_start(out=cseg_d, in_=C[:].rearrange("p a b -> p (a b)")), "cout", r=[("C", h) for h in range(4)])
    S.dma("sync", lambda e: e.dma_start(out=sseg_d, in_=ssum[:]), "sout", r=["ssum"])
    S.final_waits("sync", ["D:kTout", "D:vxo0", "D:vxo1", "D:cout", "D:sout"])
    return L.finish()


def build_ab_mlstm():
    L = Launch()
    S = L.S
    ps = L.ps
    P = launch_prologue(L)
    wq_d = L.din("wq", [4, 128, 8, 64])
    wkf_d = L.din("wkf", [4, 128, 8, 64])
    wmo_d = L.din("wmo", [128, 8, 512])
    cprev_d = L.din("cprev", [64, 3, 4 * 129])
    sprev_d = L.din("sprev", [64, 3, 4])
    msk_d = L.din("cmask", [128, 512])
    haT_d = L.dout("haT", [128, 4, TOK], BF16)
    hT = L.sb("hT", [128, 8, TOK], BF16)
    emit_hT_all(L, P, hT)
    mqT = L.sb("mqT", [64, 4, TOK], BF16)
    mkT = L.sb("mkT", [64, 4, TOK], BF16)
    emit_featmajor_proj(L, wq_d, 4, hT, mqT, "mqT", ps_ids=(4, 5), name="wfq", mc=64)
    emit_featmajor_proj(L, wkf_d, 4, hT, mkT, "mkT", ps_ids=(4, 5), name="wfk", mc=64)
    wmo = L.load("wmo", [128, 8, 512], BF16, wmo_d, "gpsimd")
    W = mlstm_weights(L)
    pools = mlstm_pools(L)
    cmask = L.load("cmask", [128, 512], F32, msk_d)
    cprev = L.load("cprev", [64, 3, 516], F32, cprev_d)
    sprev = L.load("sprev", [64, 3, 4], F32, sprev_d)
    C = L.sb("Cst", [64, 4, 129], F32)
    Cb = L.sb("Cb", [64, 4, 129], BF16)
    haT = L.sb("haT", [128, 4, TOK], BF16)
    a12 = L.sb("a12", [64, 2, 4], F32)
    e12 = L.sb("e12", [64, 2, 4], F32)
    S.op("vector", lambda e: e.tensor_copy(a12[:, 0, :], sprev[:, 0, :]), r=["sprev"], w=["a12"])
    S.op("vector", lambda e: e.tensor_tensor(a12[:, 1, :], sprev[:, 0, :], sprev[:, 1, :], ALU.add), r=["sprev", "a12"], w=["a12"])
    S.op("scalar", lambda e: e.activation(e12[:], a12[:], AF.Exp, scale=-1.0), r=["a12"], w=["e12"])
    CK = [("C", h) for h in range(4)]
    S.op("vector", lambda e: e.tensor_copy(C[:], cprev[:, 0, :].rearrange("p (a b) -> p a b", a=4)), r=["cprev"], w=CK)
    for sl in (1, 2):
        for h in range(4):
            S.op("vector", lambda e, sl=sl, h=h: e.scalar_tensor_tensor(
                C[:, h, :], cprev[:, sl, h * 129:(h + 1) * 129], e12[:, sl - 1, h:h + 1], C[:, h, :], ALU.mult, ALU.add),
                r=["cprev", "e12"] + CK, w=CK)
    SM = L.sb("SM", [128, 512], BF16)
    sig = L.sb("sig", [128, 512], F32)
    hab = L.sb("hab", [128, 512], BF16)
    den = L.sb("den", [128, 4], F32)
    nden = L.sb("nden", [128, 4], F32)
    for t in range(NT):
        col = slice(t * 128, (t + 1) * 128)
        emit_mlstm_chunk_prep(L, t, hT, W, C, pools)
        S.op("vector", lambda e: e.tensor_copy(Cb[:], C[:]), r=CK, w=["Cb"])
        emit_mlstm_state_update(L, C, pools)
        for h in range(4):
            S.op("tensor", lambda e, h=h, col=col: e.matmul(ps[4][:, h * 128:(h + 1) * 128], mkT[:, h, col], mqT[:, h, col],
                                                         start=True, stop=True),
                 r=[("mkT", h, t // 4), ("mqT", h, t // 4)], w=[("ps", 4)])
        S.op("vector", lambda e: e.tensor_tensor(SM[:], ps[4][:], cmask[:], ALU.mult), r=[("ps", 4), "cmask"], w=["SM"])
        for h in range(4):
            pair, hh, pr = h // 2, h % 2, slice((h % 2) * 64, (h % 2) * 64 + 64)
            ob = ps[5][:, hh * 129:hh * 129 + 129] if pair == 0 else ps[3][:, hh * 129:hh * 129 + 129]
            okey = ("ps", 5) if pair == 0 else ("ps", 3)
            S.op("tensor", lambda e, h=h, ob=ob: e.matmul(ob, SM[:, h * 128:(h + 1) * 128], pools["Vp"][:, h, :], start=True, stop=False),
                 r=["SM", ("Vp", h)], w=[okey])
            S.op("tensor", lambda e, h=h, ob=ob, col=col: e.matmul(ob, mqT[:, h, col], Cb[:, h, :], start=False, stop=True),
                 r=[("mqT", h, t // 4), "Cb"], w=[okey])
        for k in range(8):
            S.op("tensor", lambda e, k=k, col=col: e.matmul(ps[2][:], hT[:, k, col], wmo[:, k, :], start=(k == 0), stop=(k == 7)),
                 r=[("hT", t), "wmo"], w=[("ps", 2)])
        S.op("scalar", lambda e: e.activation(sig[:], ps[2][:], AF.Sigmoid), r=[("ps", 2)], w=["sig"])
        for pair in range(2):
            src = ps[5] if pair == 0 else ps[3]
            okey = ("ps", 5) if pair == 0 else ("ps", 3)
            S.op("vector", lambda e, pair=pair, src=src: e.tensor_copy(den[:, pair * 2:pair * 2 + 2], src[:, 128:258:129]), r=[okey], w=["den"])
        S.op("vector", lambda e: e.tensor_scalar(nden[:], den[:], -1.0, None, ALU.mult), r=["den"], w=["nden"])
        S.op("vector", lambda e: e.tensor_tensor(den[:], den[:], nden[:], ALU.max), r=["den", "nden"], w=["den"])
        S.op("vector", lambda e: e.tensor_tensor(den[:], den[:], pools["thr"][:], ALU.max), r=["den", "thr"], w=["den"])
        S.op("vector", lambda e: e.reciprocal(den[:], den[:]), r=["den"], w=["den"])
        for h in range(4):
            pair, hh = h // 2, h % 2
            src = ps[5] if pair == 0 else ps[3]
            okey = ("ps", 5) if pair == 0 else ("ps", 3)
            S.op("vector", lambda e, h=h, hh=hh, src=src: e.scalar_tensor_tensor(
                hab[:, h * 128:(h + 1) * 128], src[:, hh * 129:hh * 129 + 128], den[:, h:h + 1], sig[:, h * 128:(h + 1) * 128], ALU.mult, ALU.mult),
                r=[okey, "den", "sig"], w=["hab"])
        pTb = ps[1][:].bitcast(BF16)
        for h in range(4):
            S.op("tensor", lambda e, h=h, pTb=pTb: e.transpose(pTb[:, h * 128:(h + 1) * 128], hab[:, h * 128:(h + 1) * 128], P["ident"][:]),
                 r=["hab", "ident"], w=[("ps", 1)])
        S.op("scalar", lambda e, pTb=pTb, col=col: e.activation(haT[:, :, col], pTb[:, 0:512].rearrange("p (h t) -> p h t", h=4), AF.Copy),
             r=[("ps", 1)], w=[("haT", t)])
    S.dma("sync", lambda e: e.dma_start(out=haT_d, in_=haT[:]), "haout", r=[("haT", t) for t in range(NT)])
    S.final_waits("sync", ["D:haout"])
    return L.finish()


DIL = (1, 4, 16)


def build_ab_attn():
    L = Launch()
    S = L.S
    ps = L.ps
    vb = [L.sb(f"vb{i}", [128, 3, 32 * 65], BF16) for i in range(2)]
    stage = [(vb[i][:].rearrange("p a b -> p (a b)")[:, 0:4096].rearrange("p (k c) -> p k c", k=8), ("vb", i)) for i in range(2)]
    P = launch_prologue(L, stage=stage)
    wq_d = L.din("wqa", [8, 128, 8, 64])
    akT_d = L.din("akT", [64, 8, 2 * TOK], BF16)
    vb_d = L.din("vb", [8, 3, 128, 32 * 65], BF16)
    bias_d = L.din("biasm", [8, 128, 9 * 128])
    onesr_d = L.din("onesr", [128, 64])
    hbT_d = L.dout("hbT", [64, 8, TOK], BF16)
    hT = L.sb("hT", [128, 8, TOK], BF16)
    emit_hT_all(L, P, hT)
    aqT = L.sb("aqT", [64, 8, TOK], BF16)
    emit_featmajor_proj(L, wq_d, 8, hT, aqT, "aqT", ps_ids=(4, 5), name="wfq", mc=64)
    akTb = [L.sb(f"akT{i}", [64, 2 * TOK], BF16) for i in range(2)]
    onesr = L.load("onesr", [128, 64], F32, onesr_d)
    hbT = L.sb("hbT", [64, 8, TOK], BF16)
    biasf = L.sb("biasf", [128, 9 * 128], F32)
    E = [L.sb(f"E{i}", [128, 9, 128], BF16) for i in range(2)]
    NPB = 3
    Pe = [L.sb(f"Pe{i}", [128, 512], BF16) for i in range(NPB)]
    PT = [L.sb(f"PT{i}", [128, 512], BF16) for i in range(NPB)]
    lrow = L.sb("lrow", [128, 512], F32)
    bcs = L.sb("bcs", [64, 512], F32)
    AQ = [("aqT", c, pc) for c in range(8) for pc in range(4)]
    l0 = L.sb("l0", [1, 512], F32)
    OWN = TOK
    cnt = {"s": 0}
    for h in range(8):
        vbt, Et, akT = vb[h % 2], E[h % 2], akTb[h % 2]
        S.dma("sync", lambda e, akT=akT, h=h: e.dma_start(out=akT[:], in_=akT_d[:, h, :]), f"akT{h % 2}", w=[("akT", h % 2)])
        for br in range(3):
            S.dma("sync", lambda e, br=br, vbt=vbt, h=h: e.dma_start(out=vbt[:, br, :], in_=vb_d[h, br]), f"vb{h % 2}", w=[("vb", h % 2)])
        S.dma("sync", lambda e, h=h: e.dma_start(out=biasf[:], in_=bias_d[h]), "biasf", w=["biasf"])
        S.op("scalar", lambda e, Et=Et: e.activation(Et[:].rearrange("p a b -> p (a b)"), biasf[:], AF.Exp), r=["biasf"], w=[("E", h % 2)])

        def score_group(items):
            i = cnt["s"]
            cnt["s"] += 1
            bank = 4 + i % 2
            for j, (kc, qc, ek) in enumerate(items):
                S.op("tensor", lambda e, j=j, kc=kc, qc=qc, bank=bank, h=h, akT=akT: e.matmul(ps[bank][:, j * 128:(j + 1) * 128], akT[:, kc], aqT[:, h, qc],
                                                                             start=True, stop=True),
                     r=[("akT", h % 2)] + AQ, w=[("ps", bank)])
            pe, pt = Pe[i % NPB], PT[i % NPB]
            n = len(items) * 128
            S.op("scalar", lambda e, pe=pe, bank=bank, n=n: e.activation(pe[:, :n], ps[bank][:, :n], AF.Exp, scale=0.125), r=[("ps", bank)], w=[("Pe", i % NPB)])
            for j, (kc, qc, ek) in enumerate(items):
                S.op("vector", lambda e, j=j, ek=ek, pe=pe, pt=pt, Et=Et: e.tensor_tensor(pt[:, j * 128:(j + 1) * 128], pe[:, j * 128:(j + 1) * 128], Et[:, ek, :], ALU.mult),
                     r=[("Pe", i % NPB), ("E", h % 2)], w=[("PT", i % NPB, j)])
            return pt, ("PT", i % NPB)

        def pv(pt, ptkey, j0, ncols, blk_br, blk, sp, ocols, first, last):
            S.op("tensor", lambda e, vbt=vbt: e.matmul(ps[sp][0:65, ocols], vbt[:, blk_br, blk * 65:(blk + 1) * 65], pt[:, j0:j0 + ncols],
                                              start=first, stop=last, skip_group_check=True),
                 r=[ptkey + (j0 // 128,), ("vb", h % 2)], w=[("ps", sp)])

        for sp in range(4):
            for kind in (0, 1):
                items = []
                for a in range(4):
                    qb = sp * 4 + a
                    qc = slice(qb * 128, (qb + 1) * 128)
                    if kind == 0:
                        kc, ek = slice(OWN + qb * 128, OWN + (qb + 1) * 128), 0
                    else:
                        kc = slice(OWN + (qb - 1) * 128, OWN + qb * 128)
                        ek = 1 if qb >= 1 else 2
                    items.append((kc, qc, ek))
                pt, ptk = score_group(items)
                for a in range(4):
                    qb = sp * 4 + a
                    blk = 16 + qb if kind == 0 else 16 + qb - 1
                    pv(pt, ptk, a * 128, 128, 0, blk, sp, slice(a * 128, (a + 1) * 128), first=(kind == 0 and a == 0), last=False)
        for sp in range(4):
            for kind in (0, 1):
                items = []
                for r in range(4):
                    qc = slice(512 * sp + r, 512 * sp + 512, 4)
                    ksp = sp if kind == 0 else sp - 1
                    kc = slice(OWN + 512 * ksp + r, OWN + 512 * ksp + 512, 4)
                    ek = 3 if kind == 0 else (4 if sp >= 1 else 5)
                    items.append((kc, qc, ek))
                pt, ptk = score_group(items)
                for r in range(4):
                    blk = (4 + sp) * 4 + r if kind == 0 else (4 + sp - 1) * 4 + r
                    pv(pt, ptk, r * 128, 128, 1, blk, sp, slice(r, 512, 4), first=False, last=False)
        for rg in range(4):
            for kind in (0, 1):
                items = []
                for a in range(4):
                    r = rg * 4 + a
                    qc = slice(r, TOK, 16)
                    kc = slice(OWN + r, OWN + TOK, 16) if kind == 0 else slice(r, TOK, 16)
                    items.append((kc, qc, 6 if kind == 0 else 8))
                pt, ptk = score_group(items)
                for a in range(4):
                    r = rg * 4 + a
                    blk = 16 + r if kind == 0 else r
                    for sp in range(4):
                        pv(pt, ptk, a * 128 + 32 * sp, 32, 2, blk, sp, slice(r, 512, 16), first=False, last=(kind == 1))
        for sp in range(4):
            S.op("scalar", lambda e, sp=sp: e.activation(lrow[64:65, :], ps[sp][64:65, :], AF.Copy), r=[("ps", sp)], w=["lrow"])
            S.dma("sync", lambda e: e.dma_start(out=l0[:], in_=lrow[64:65, :]), "l0", r=["lrow"], w=["l0"])
            S.op("vector", lambda e: e.reciprocal(l0[:], l0[:]), r=["l0"], w=["l0"])
            bank = 6 + sp % 2
            S.op("tensor", lambda e, bank=bank: e.matmul(ps[bank][0:64, :], onesr[0:1, :], l0[:], start=True, stop=True),
                 r=["l0", "onesr"], w=[("ps", bank)])
            S.op("scalar", lambda e, bank=bank: e.activation(bcs[:], ps[bank][0:64, :], AF.Copy), r=[("ps", bank)], w=["bcs"])
            S.op("vector", lambda e, sp=sp, h=h: e.tensor_tensor(hbT[:, h, sp * 512:(sp + 1) * 512], ps[sp][0:64, :], bcs[:], ALU.mult),
                 r=[("ps", sp), "bcs"], w=[("hbT", h)])
    S.dma("sync", lambda e: e.dma_start(out=hbT_d, in_=hbT[:]), "hbout", r=[("hbT", h) for h in range(8)])
    S.final_waits("sync", ["D:hbout"])
    return L.finish()


def build_ab_out():
    L = Launch()
    S = L.S
    ps = L.ps
    P = launch_prologue(L, want_ln=True)
    haT_d = L.din("haT", [128, 4, TOK], BF16)
    hbT_d = L.din("hbT", [64, 8, TOK], BF16)
    woa_d = L.din("woa", [128, 4, D])
    wob_d = L.din("wob", [64, 8, D])
    y_d = L.dout("y", [TOK, D])
    haT = L.load("haT", [128, 4, TOK], BF16, haT_d)
    hbT = L.load("hbT", [64, 8, TOK], BF16, hbT_d)
    woa = L.load("woa", [128, 4, D], BF16, woa_d, "gpsimd")
    wob = L.load("wob", [64, 8, D], BF16, wob_d, "gpsimd")
    tmp = [L.sb(f"tmp{i}", [128, D], F32) for i in range(2)]
    xin = [L.sb(f"xin{i}", [128, D], F32) for i in range(2)]
    st6 = L.sb("st6", [128, 2, 6], F32)
    mv = L.sb("mv", [128, 2], F32)
    rstd = L.sb("rstd", [128, 1], F32)
    for t in range(NT):
        xt = xin[t % 2]
        col = slice(t * 128, (t + 1) * 128)
        S.dma("sync", lambda e, t=t, xt=xt: e.dma_start(out=xt[:], in_=P["x_d"][t * 128:(t + 1) * 128, :]), f"xin{t % 2}", w=[("xin", t % 2)])
        pa = (t % 2) * 2
        for dh in range(2):
            dc = slice(dh * 512, (dh + 1) * 512)
            for k in range(4):
                S.op("tensor", lambda e, k=k, dc=dc, col=col, pa=pa, dh=dh: e.matmul(ps[pa + dh][:], haT[:, k, col], woa[:, k, dc], start=(k == 0), stop=False),
                     r=["haT", "woa"], w=[("ps", pa + dh)])
            for k in range(8):
                S.op("tensor", lambda e, k=k, dc=dc, col=col, pa=pa, dh=dh: e.matmul(ps[pa + dh][:], hbT[:, k, col], wob[:, k, dc], start=False, stop=(k == 7)),
                     r=["hbT", "wob"], w=[("ps", pa + dh)])
        tm = tmp[t % 2]
        for dh in range(2):
            S.op("vector", lambda e, tm=tm, dh=dh, pa=pa: e.tensor_tensor(
                tm[:, dh * 512:(dh + 1) * 512], ps[pa + dh][:], P["gv"][:, dh * 512:(dh + 1) * 512], ALU.mult),
                r=[("ps", pa + dh)] + P["MODR"], w=[("tmp", t % 2)])
        S.op("vector", lambda e, tm=tm, xt=xt: e.scalar_tensor_tensor(xt[:], xt[:], ALPHA, tm[:], ALU.mult, ALU.add),
             r=[("tmp", t % 2), ("xin", t % 2)], w=[("xin", t % 2)])
        emit_ln(S, xt[:], ("xin", t % 2), st6, mv, rstd, P["lng"], P["lnb"])
        S.dma("sync", lambda e, t=t, xt=xt: e.dma_start(out=y_d[t * 128:(t + 1) * 128, :], in_=xt[:]), f"yo{t % 2}", r=[("xin", t % 2)])
    S.final_waits("sync", ["D:yo0", "D:yo1"])
    return L.finish()


_PROGS = {}


def _prog(name, builder):
    if name not in _PROGS:
        _PROGS[name] = builder()
    return _PROGS[name]


def _run(name, builder, in_maps):
    nc = _prog(name, builder)
    res = run_bass_kernel_spmd(nc, in_maps, core_ids=list(range(NCORES)))
    return res.results


def _rel_bucket(dist):
    max_exact = 16
    d = np.maximum(dist, 0)
    large = max_exact + (np.log(np.maximum(d, 1).astype(np.float32) / np.float32(max_exact))
                         / np.float32(np.log(2048 / max_exact)) * np.float32(32 - max_exact)).astype(np.int32)
    large = np.minimum(large, 31)
    return np.where(d < max_exact, d, large)


def _bias_tables(rel_bias, has_prev):
    NEG = np.float32(-30000.0)
    ik = np.arange(128)[:, None]
    iq = np.arange(128)[None, :]
    out = np.full((8, 128, 9, 128), NEG, np.float32)
    for bi, d in enumerate(DIL):
        m_same = iq - ik
        m_prev = iq + 128 - ik
        b_same = rel_bias[_rel_bucket(m_same * d)]
        b_prev = rel_bias[_rel_bucket(m_prev * d)]
        for h in range(8):
            out[h, :, bi * 3 + 0, :] = np.where(m_same >= 0, b_same[:, :, h], NEG)
            pv = np.where(m_prev <= 128, b_prev[:, :, h], NEG)
            out[h, :, bi * 3 + 1, :] = pv
            out[h, :, bi * 3 + 2, :] = pv if has_prev else NEG
    return out.reshape(8, 128, 9 * 128)


def _vb_layout(V):
    V4 = V.reshape(4096, 8, 65)
    o = np.empty((8, 3, 128, 32, 65), V.dtype)
    o[:, 0] = V4.reshape(32, 128, 8, 65).transpose(2, 1, 0, 3)
    o[:, 1] = V4.reshape(8, 128, 4, 8, 65).transpose(3, 1, 0, 2, 4).reshape(8, 128, 32, 65)
    o[:, 2] = V4.reshape(2, 128, 16, 8, 65).transpose(3, 1, 0, 2, 4).reshape(8, 128, 32, 65)
    return np.ascontiguousarray(o.reshape(8, 3, 128, 32 * 65))


def kernel(x, c, rel_bias, ada_w, ada_b, ln_g, ln_b, ffn_w_gate, ffn_w_up, ffn_w_down,
           ab_w_in, ab_w_out, ab_b_igate, ab_b_fgate,
           cd_w_in, cd_w_out, cd_conv_w, cd_conv_b, cd_sgu_ln_g, cd_sgu_ln_b, cd_sgu_w, cd_sgu_b, _debug=None):
    f32 = np.float32
    A = lambda a: np.asarray(a, dtype=f32)
    x, c, rel_bias, ada_w, ada_b, ln_g, ln_b = map(A, (x, c, rel_bias, ada_w, ada_b, ln_g, ln_b))
    ffn_w_gate, ffn_w_up, ffn_w_down = map(A, (ffn_w_gate, ffn_w_up, ffn_w_down))
    ab_w_in, ab_w_out, ab_b_igate, ab_b_fgate = map(A, (ab_w_in, ab_w_out, ab_b_igate, ab_b_fgate))
    cd_w_in, cd_w_out, cd_conv_w, cd_conv_b = map(A, (cd_w_in, cd_w_out, cd_conv_w, cd_conv_b))
    cd_sgu_ln_g, cd_sgu_ln_b, cd_sgu_w, cd_sgu_b = map(A, (cd_sgu_ln_g, cd_sgu_ln_b, cd_sgu_w, cd_sgu_b))
    cores = [(r // 4, r % 4) for r in range(NCORES)]
    xs = [x[b, j * TOK:(j + 1) * TOK] for (b, j) in cores]

    def ffn_stage(xs, layer, slot):
        s = 0 if slot == 0 else 2
        wg, wu, wd = lay_w_in(ffn_w_gate[layer, slot]), lay_w_in(ffn_w_up[layer, slot]), np.ascontiguousarray(ffn_w_down[layer, slot].reshape(NCH, 128, D))
        maps = []
        for r, (b, j) in enumerate(cores):
            m = mod_inputs(c[b], ada_w[layer], ada_b[layer], s, ln_g[layer, s], ln_b[layer, s])
            m.update({"x": np.ascontiguousarray(xs[r]), "wg": wg, "wu": wu, "wd": wd})
            maps.append(m)
        res = _run("ffn", build_ffn, maps)
        return [res[r]["y"] for r in range(NCORES)]

    def dbg(name, xs):
        if _debug is not None:
            _debug[name] = np.stack(xs).reshape(2, SEQ, D).copy()

    xs = ffn_stage(xs, 0, 0)
    dbg("l0s0", xs)
    w = ab_w_in[0]
    tri = (np.arange(128)[:, None] <= np.arange(128)[None, :]).astype(f32)
    common_m = {
        "wmk": lay_cols_rhs(w[:, 256:512]), "wmv": lay_cols_rhs(w[:, 512:1024]), "wgt": lay_cols_rhs(w[:, 1536:1544]),
        "bgate": np.ascontiguousarray(np.broadcast_to(np.concatenate([ab_b_igate[0], ab_b_fgate[0]])[None, :], (128, 8))),
        "tri": tri, "onesf": np.ones((128, 128), f32),
    }
    mods1 = [mod_inputs(c[b], ada_w[0], ada_b[0], 1, ln_g[0, 1], ln_b[0, 1]) for (b, j) in cores]
    maps = []
    for r in range(NCORES):
        m = dict(mods1[r]); m.update(common_m)
        m.update({"x": np.ascontiguousarray(xs[r]), "wk": lay_w_in(w[:, 2056:2568], 64), "wav": lay_cols_rhs(w[:, 2568:3080])})
        maps.append(m)
    prod = _run("abp", build_ab_producer, maps)
    if _debug is not None:
        _debug["prod"] = prod
    maps = []
    cmask = np.tile((np.arange(128)[None, :] >= np.arange(128)[:, None]).astype(f32), (1, 4))
    for r, (b, j) in enumerate(cores):
        cprev = np.zeros((64, 3, 516), f32)
        sprev = np.zeros((64, 3, 4), f32)
        for k in range(3):
            if j - 1 - k >= 0:
                cprev[:, k] = prod[r - 1 - k]["cseg"]
                sprev[:, k] = prod[r - 1 - k]["sseg"]
        m = dict(mods1[r]); m.update(common_m)
        m.update({"x": np.ascontiguousarray(xs[r]), "wq": lay_w_in(w[:, 0:256], 64), "wkf": lay_w_in(w[:, 256:512], 64),
                  "wmo": lay_cols_rhs(w[:, 1024:1536]), "cprev": cprev, "sprev": sprev, "cmask": cmask})
        maps.append(m)
    ml = _run("abm", build_ab_mlstm, maps)
    maps = []
    for r, (b, j) in enumerate(cores):
        own_k, own_v = prod[r]["kT"], prod[r]["vx"]
        if j > 0:
            pk, pvx = prod[r - 1]["kT"], prod[r - 1]["vx"]
        else:
            pk, pvx = np.zeros_like(own_k), np.zeros_like(own_v)
        m = dict(mods1[r])
        m.update({"x": np.ascontiguousarray(xs[r]), "wqa": lay_w_in(w[:, 1544:2056], 64),
                  "akT": np.ascontiguousarray(np.concatenate([pk, own_k], axis=2)),
                  "vb": _vb_layout(np.concatenate([pvx, own_v], axis=0)),
                  "biasm": _bias_tables(rel_bias, j > 0), "onesr": np.ones((128, 64), f32)})
        maps.append(m)
    at = _run("aba", build_ab_attn, maps)
    if _debug is not None:
        _debug["haT"] = [ml[r]["haT"] for r in range(NCORES)]
        _debug["hbT"] = [at[r]["hbT"] for r in range(NCORES)]
    maps = []
    wo = ab_w_out[0]
    for r in range(NCORES):
        m = dict(mods1[r])
        m.update({"x": np.ascontiguousarray(xs[r]), "haT": ml[r]["haT"], "hbT": at[r]["hbT"],
                  "woa": np.ascontiguousarray(wo[0:512].reshape(4, 128, D).transpose(1, 0, 2)),
                  "wob": np.ascontiguousarray(wo[512:1024].reshape(8, 64, D).transpose(1, 0, 2))})
        maps.append(m)
    res = _run("abo", build_ab_out, maps)
    xs = [res[r]["y"] for r in range(NCORES)]
    dbg("l0s1", xs)
    xs = ffn_stage(xs, 0, 1)
    dbg("l0s2", xs)
    xs = ffn_stage(xs, 1, 0)
    dbg("l1s0", xs)
    maps = []
    for r, (b, j) in enumerate(cores):
        halo = xs[r - 1][TOK - 128:] if j > 0 else np.zeros((128, D), f32)
        maps.append(cd_inputs(xs[r], halo, j > 0, c[b], ada_w[1], ada_b[1], ln_g[1, 1], ln_b[1, 1], cd_w_in[0], cd_w_out[0],
                              cd_conv_w[0], cd_conv_b[0], cd_sgu_ln_g[0], cd_sgu_ln_b[0], cd_sgu_w[0], cd_sgu_b[0]))
    res = _run("cd", build_cd, maps)
    xs = [res[r]["y"] for r in range(NCORES)]
    dbg("l1s1", xs)
    xs = ffn_stage(xs, 1, 1)
    return np.stack(xs).reshape(2, SEQ, D).astype(np.float32)
```
